# Optimizing a Trainium2 kernel written in Bass

```python
import math
import jax, jax.numpy as jnp
from jax import lax
import numpy as np

D_MODEL = 1024
BATCH = 32
SEQ = 2048
DEPTH = 2

N_EVEN = (DEPTH + 1) // 2
N_ODD = DEPTH // 2

D_FF = 2816
FFN_RES = 0.5
NORM_EPS = 1e-6

POOL_WINDOWS = (2, 4, 8, 16)
N_POOL_GROUPS = 4
POOL_WIDTH = D_MODEL // 2
POOL_GROUP = POOL_WIDTH // N_POOL_GROUPS

CONV_WIDTH = D_MODEL // 2
CONV_KERNEL = 31

AB_IN = POOL_WIDTH + 2 * CONV_WIDTH
AB_OUT = POOL_WIDTH + CONV_WIDTH

N_HEADS = 16
HEAD_DIM = 64
ATTN_WIDTH = N_HEADS * HEAD_DIM
N_IDX_HEADS = 8
IDX_DIM = 64
TOPK_MAX = 256
Q_BLOCK = 128
C_SPLITS = (ATTN_WIDTH, ATTN_WIDTH, ATTN_WIDTH, N_IDX_HEADS * IDX_DIM, IDX_DIM, N_IDX_HEADS)
C_IN = sum(C_SPLITS)

REL_BUCKETS = 32
REL_MAX_DIST = 128

kernel_name = 'hybrid_pool_conv_dsa_macaron'


def _offsets(sizes):
    out, acc = [], 0
    for s in sizes[:-1]:
        acc += s
        out.append(acc)
    return out


def rms_norm(x, g):
    xf = x.astype(jnp.float32)
    y = xf * lax.rsqrt(jnp.mean(xf * xf, axis=-1, keepdims=True) + NORM_EPS)
    return (y * g.astype(jnp.float32)).astype(x.dtype)


def swiglu(h, w_gate, w_up, w_down):
    return (jax.nn.silu(h @ w_gate) * (h @ w_up)) @ w_down


def pool_mixer(u, pool_w, pool_scale):
    B, S, _ = u.shape
    uf = u.astype(jnp.float32).reshape(B, S, N_POOL_GROUPS, POOL_GROUP)
    c0 = jnp.concatenate([jnp.zeros((B, 1, N_POOL_GROUPS, POOL_GROUP), jnp.float32),
                          jnp.cumsum(uf, axis=1)], axis=1)
    t = jnp.arange(S, dtype=jnp.int32)[:, None]
    win = jnp.array(POOL_WINDOWS, dtype=jnp.int32)[None, :]
    lo = jnp.maximum(t + 1 - win, 0)
    g_idx = jnp.arange(N_POOL_GROUPS, dtype=jnp.int32)[None, :]
    lo_sum = c0[:, lo, g_idx, :]
    count = (t + 1 - lo).astype(jnp.float32)
    mean = (c0[:, 1:] - lo_sum) / count[None, :, :, None]
    y = jnp.einsum('bsgc,gcd->bsgd', mean - uf, pool_w.astype(jnp.float32))
    y = y * pool_scale.astype(jnp.float32).reshape(N_POOL_GROUPS, POOL_GROUP)
    return y.reshape(B, S, POOL_WIDTH).astype(u.dtype)


def conv_module(a, gate, conv_w, conv_b, ln_g, ln_b):
    z = a * jax.nn.sigmoid(gate)
    z = lax.conv_general_dilated(
        z, conv_w[:, None, :].astype(z.dtype), window_strides=(1,),
        padding=[(CONV_KERNEL - 1, 0)], dimension_numbers=('NWC', 'WIO', 'NWC'),
        feature_group_count=CONV_WIDTH) + conv_b
    zf = z.astype(jnp.float32)
    mu = jnp.mean(zf, axis=-1, keepdims=True)
    var = jnp.mean(jnp.square(zf - mu), axis=-1, keepdims=True)
    zf = (zf - mu) * lax.rsqrt(var + NORM_EPS) * ln_g.astype(jnp.float32) + ln_b.astype(jnp.float32)
    return jax.nn.silu(zf).astype(a.dtype)


def t5_bucket(dist):
    max_exact = REL_BUCKETS // 2
    d = jnp.maximum(dist, max_exact).astype(jnp.float32)
    large = max_exact + (jnp.log(d / max_exact) / math.log(REL_MAX_DIST / max_exact)
                         * (REL_BUCKETS - max_exact)).astype(jnp.int32)
    large = jnp.minimum(large, REL_BUCKETS - 1)
    return jnp.where(dist < max_exact, dist, large)


def dsa_attention(q, k, v, qi, ki, wi, rel_bias):
    S = q.shape[1]
    top_k = min(TOPK_MAX, S // 4)
    nb = S // Q_BLOCK
    pos = jnp.arange(S, dtype=jnp.int32)
    idx_scale = (N_IDX_HEADS * IDX_DIM) ** -0.5
    attn_scale = HEAD_DIM ** -0.5

    def one_seq(args):
        qb, kb, vb, qib, kib, wib = args
        kib32 = kib.astype(jnp.float32)

        def one_block(bargs):
            q_t, qi_t, wi_t, t = bargs
            rel = jax.nn.relu(jnp.einsum('thd,sd->ths', qi_t.astype(jnp.float32), kib32))
            score = jnp.einsum('ths,th->ts', rel, wi_t.astype(jnp.float32)) * idx_scale
            score = jnp.where(pos[None, :] <= t[:, None], score, -jnp.inf)
            _, sel = lax.top_k(score, top_k)
            k_sel = kb[sel]
            v_sel = vb[sel]
            logits = jnp.einsum('thd,tkhd->thk', q_t, k_sel).astype(jnp.float32) * attn_scale
            bias = rel_bias[t5_bucket(t[:, None] - sel)]
            logits = logits + jnp.transpose(bias, (0, 2, 1)).astype(jnp.float32)
            logits = jnp.where((sel <= t[:, None])[:, None, :], logits, -jnp.inf)
            p = jax.nn.softmax(logits, axis=-1).astype(v_sel.dtype)
            return jnp.einsum('thk,tkhd->thd', p, v_sel)

        blocks = (qb.reshape(nb, Q_BLOCK, N_HEADS, HEAD_DIM),
                  qib.reshape(nb, Q_BLOCK, N_IDX_HEADS, IDX_DIM),
                  wib.reshape(nb, Q_BLOCK, N_IDX_HEADS),
                  pos.reshape(nb, Q_BLOCK))
        out = lax.map(one_block, blocks)
        return out.reshape(S, ATTN_WIDTH)

    return lax.map(one_seq, (q, k, v, qi, ki, wi))


def setup_inputs(seed: int = 0) -> dict:
    key = jax.random.key(seed)
    ks = jax.random.split(key, 24)
    f32 = jnp.float32

    def w(k, shape, fan_in):
        return jax.random.normal(k, shape, f32) * (fan_in ** -0.5)

    def gain(k, shape):
        return 1.0 + 0.02 * jax.random.normal(k, shape, f32)

    def small(k, shape):
        return 0.01 * jax.random.normal(k, shape, f32)

    return {
        'x': jax.random.normal(ks[0], (BATCH, SEQ, D_MODEL), f32),
        'ffn1_norm': gain(ks[1], (DEPTH, D_MODEL)),
        'ffn1_w_gate': w(ks[2], (DEPTH, D_MODEL, D_FF), D_MODEL),
        'ffn1_w_up': w(ks[3], (DEPTH, D_MODEL, D_FF), D_MODEL),
        'ffn1_w_down': w(ks[4], (DEPTH, D_FF, D_MODEL), D_FF),
        'mix_norm': gain(ks[5], (DEPTH, D_MODEL)),
        'ffn2_norm': gain(ks[6], (DEPTH, D_MODEL)),
        'ffn2_w_gate': w(ks[7], (DEPTH, D_MODEL, D_FF), D_MODEL),
        'ffn2_w_up': w(ks[8], (DEPTH, D_MODEL, D_FF), D_MODEL),
        'ffn2_w_down': w(ks[9], (DEPTH, D_FF, D_MODEL), D_FF),
        'ab_w_in': w(ks[10], (N_EVEN, D_MODEL, AB_IN), D_MODEL),
        'pool_w': w(ks[11], (N_EVEN, N_POOL_GROUPS, POOL_GROUP, POOL_GROUP), POOL_GROUP),
        'pool_scale': gain(ks[12], (N_EVEN, POOL_WIDTH)),
        'conv_w': w(ks[13], (N_EVEN, CONV_KERNEL, CONV_WIDTH), CONV_KERNEL),
        'conv_b': small(ks[14], (N_EVEN, CONV_WIDTH)),
        'conv_ln_g': gain(ks[15], (N_EVEN, CONV_WIDTH)),
        'conv_ln_b': small(ks[16], (N_EVEN, CONV_WIDTH)),
        'ab_w_out': w(ks[17], (N_EVEN, AB_OUT, D_MODEL), AB_OUT),
        'c_w_in': w(ks[18], (N_ODD, D_MODEL, C_IN), D_MODEL),
        'c_w_out': w(ks[19], (N_ODD, ATTN_WIDTH, D_MODEL), ATTN_WIDTH),
        'rel_bias': 0.1 * jax.random.normal(ks[20], (REL_BUCKETS, N_HEADS), f32),
        'final_norm': gain(ks[21], (D_MODEL,)),
    }


def reference(x, ffn1_norm, ffn1_w_gate, ffn1_w_up, ffn1_w_down, mix_norm, ffn2_norm,
              ffn2_w_gate, ffn2_w_up, ffn2_w_down, ab_w_in, pool_w, pool_scale, conv_w,
              conv_b, conv_ln_g, conv_ln_b, ab_w_out, c_w_in, c_w_out, rel_bias, final_norm):
    B, S, _ = x.shape
    ab_cuts = _offsets((POOL_WIDTH, CONV_WIDTH, CONV_WIDTH))
    c_cuts = _offsets(C_SPLITS)
    for layer in range(DEPTH):
        h = rms_norm(x, ffn1_norm[layer])
        x = x + FFN_RES * swiglu(h, ffn1_w_gate[layer], ffn1_w_up[layer], ffn1_w_down[layer])
        h = rms_norm(x, mix_norm[layer])
        i = layer // 2
        if layer % 2 == 0:
            proj = h @ ab_w_in[i]
            u_pool, u_val, u_gate = jnp.split(proj, ab_cuts, axis=-1)
            y_pool = pool_mixer(u_pool, pool_w[i], pool_scale[i])
            y_conv = conv_module(u_val, u_gate, conv_w[i], conv_b[i], conv_ln_g[i], conv_ln_b[i])
            y = jnp.concatenate([y_pool, y_conv], axis=-1) @ ab_w_out[i]
        else:
            proj = h @ c_w_in[i]
            q, k, v, qi, ki, wi = jnp.split(proj, c_cuts, axis=-1)
            attn = dsa_attention(q.reshape(B, S, N_HEADS, HEAD_DIM),
                                 k.reshape(B, S, N_HEADS, HEAD_DIM),
                                 v.reshape(B, S, N_HEADS, HEAD_DIM),
                                 qi.reshape(B, S, N_IDX_HEADS, IDX_DIM), ki, wi, rel_bias)
            y = attn @ c_w_out[i]
        x = x + y
        h = rms_norm(x, ffn2_norm[layer])
        x = x + FFN_RES * swiglu(h, ffn2_w_gate[layer], ffn2_w_up[layer], ffn2_w_down[layer])
    return rms_norm(x, final_norm)
```

```python
import math, os
from contextlib import ExitStack
import numpy as np
import concourse.bass as bass
import concourse.mybir as mybir
from concourse.bass_utils import run_bass_kernel_spmd

F32 = mybir.dt.float32
BF16 = mybir.dt.bfloat16
ALU = mybir.AluOpType
AF = mybir.ActivationFunctionType
AX = mybir.AxisListType

D = 1024
S = 2048
DFF = 2816
NF = 22
CH = 1024
NTT = CH // 512
EPS = 1e-6
NIT = 12
FVL = 1152
NEG = -1.0e30

C_ID, C_J, C_CM, C_ONE, C_P2, C_IC = 0, 128, 256, 384, 512, 528
NCONST = 544
V_G = 0
V_PS = 56
V_CW = 60
V_CB = 184
V_LG = 188
V_LB = 192
V_B31 = 196
NVEC = 212


class Node:
    __slots__ = ("eng", "fn", "deps", "sig", "val", "dma", "semi", "waits")


class Sched:
    COMPUTE = ("pe", "act", "dve", "pool")
    KD = 8

    def __init__(self):
        self.nodes = []
        self.recs = {}

    def add(self, eng, fn, reads=(), writes=(), dma=False):
        nid = len(self.nodes)
        n = Node()
        n.eng, n.fn, n.dma, n.sig, n.val, n.semi = eng, fn, dma, False, 0, -1
        deps = {}
        recs = self.recs
        for (b, lo, hi) in reads:
            for r in recs.get(b, ()):
                if r[3] and r[0] < hi and lo < r[1]:
                    deps[r[2]] = True
        for (b, lo, hi) in writes:
            for r in recs.get(b, ()):
                if r[0] < hi and lo < r[1] and r[2] not in deps:
                    deps[r[2]] = False
        keep = []
        nodes = self.nodes
        for d, raw in deps.items():
            dn = nodes[d]
            if dn.eng == eng and not dn.dma and not dma:
                if eng == "pe":
                    continue
            keep.append(d)
        n.deps = keep
        for (b, lo, hi) in writes:
            L = recs.setdefault(b, [])
            L[:] = [r for r in L if not (lo <= r[0] and r[1] <= hi)]
            L.append((lo, hi, nid, True))
        for (b, lo, hi) in reads:
            L = recs.setdefault(b, [])
            if not dma:
                L[:] = [r for r in L if not ((not r[3]) and lo <= r[0] and r[1] <= hi
                                             and nodes[r[2]].eng == eng and not nodes[r[2]].dma)]
            L.append((lo, hi, nid, False))
        nodes.append(n)
        return nid

    def finalize(self):
        nodes = self.nodes
        for n in nodes:
            for d in n.deps:
                nodes[d].sig = True
        cnt = {e: 0 for e in self.COMPUTE}
        dcnt = {}
        for n in nodes:
            if n.dma:
                j = dcnt.get(n.eng, 0)
                dcnt[n.eng] = j + 1
                n.semi = j % self.KD
                n.val = 16 * (j // self.KD + 1)
            elif n.sig:
                cnt[n.eng] += 1
                n.val = cnt[n.eng]
        waited = {}
        for n in nodes:
            w = {}
            for d in n.deps:
                dn = nodes[d]
                key = ("d", dn.eng, dn.semi) if dn.dma else ("c", dn.eng)
                if dn.val > w.get(key, 0):
                    w[key] = dn.val
            if n.dma and n.val > 16:
                key = ("d", n.eng, n.semi)
                w[key] = max(w.get(key, 0), n.val - 16)
            out = []
            for key, v in w.items():
                wk = (n.eng, key)
                if waited.get(wk, 0) >= v:
                    continue
                waited[wk] = v
                out.append((key, v))
            n.waits = out

    def emit(self, nc, block, sems):
        engmap = {"pe": block.tensor, "act": block.scalar, "dve": block.vector,
                  "pool": block.gpsimd, "sp": block.sync}
        nodes = self.nodes
        for ename, deco in engmap.items():
            mine = [n for n in nodes if n.eng == ename]
            if not mine:
                continue

            def body(e, mine=mine, ename=ename):
                for n in mine:
                    for key, v in n.waits:
                        e.wait_ge(sems[key], v)
                    ins = n.fn(e)
                    if n.dma:
                        ins.then_inc(sems[("d", ename, n.semi)], 16)
                    elif n.sig:
                        ins.then_inc(sems[("c", ename)], 1)
                last = {}
                for n in mine:
                    if n.dma:
                        last[n.semi] = n.val
                for semi, v in last.items():
                    e.wait_ge(sems[("d", ename, semi)], v)
            deco(body)


class V:
    __slots__ = ("ap", "reg")

    def __init__(self, ap, reg):
        self.ap, self.reg = ap, reg


class Buf:
    def __init__(self, space, base_ap, off_words, shape, dtype):
        self.space = space
        self.esz = 4 if dtype == F32 else 2
        self.shape = tuple(shape)
        nel = int(np.prod(shape))
        self.words = (nel * self.esz + 3) // 4
        self.off = off_words
        v = base_ap[:, off_words:off_words + self.words]
        if dtype != F32:
            v = v.bitcast(dtype)
        if len(shape) == 2:
            v = v.rearrange("p (a b) -> p a b", a=shape[0])
        elif len(shape) == 3:
            v = v.rearrange("p (a b c) -> p a b c", a=shape[0], b=shape[1])
        self.ap = v
        st = []
        acc = 1
        for s_ in reversed(shape):
            st.append(acc)
            acc *= s_
        self.strides = tuple(reversed(st))

    def v(self, *idx, p=None):
        lo = 0
        hi = 0
        key = []
        for i, s_ in enumerate(self.shape):
            it = idx[i] if i < len(idx) else None
            if it is None:
                a, b = 0, s_
                key.append(slice(None))
            elif isinstance(it, tuple):
                a, b = it
                key.append(slice(a, b))
            else:
                a, b = it, it + 1
                key.append(it)
            lo += a * self.strides[i]
            hi += (b - 1) * self.strides[i]
        hi += 1
        ps = slice(None) if p is None else slice(p[0], p[1])
        ap = self.ap[(ps,) + tuple(key)]
        reg = (self.space, self.off + (lo * self.esz) // 4, self.off + (hi * self.esz + 3) // 4)
        return V(ap, reg)


class Prog:
    def __init__(self, n_seq, stages, do_final):
        self.n_seq = n_seq
        self.stages = stages
        self.do_final = do_final
        self.S = Sched()
        self.nc = bass.Bass("TRN2", target_bir_lowering=False)
        self.psi = 0
        self.units = []
        self.rr = 0

    def mm(self, out, lhsT, rhs, start, stop):
        self.S.add("pe", lambda e, o=out.ap, l=lhsT.ap, r=rhs.ap, a=start, b=stop:
                   e.matmul(o, l, r, start=a, stop=b),
                   reads=[lhsT.reg, rhs.reg], writes=[out.reg])

    def tr(self, out, in_, ident):
        self.S.add("pe", lambda e, o=out.ap, i=in_.ap, d=ident.ap: e.transpose(o, i, d),
                   reads=[in_.reg, ident.reg], writes=[out.reg])

    def act(self, out, in_, func, bias=0.0, scale=1.0, accum=None, eng="act"):
        reads = [in_.reg]
        b = bias
        s = scale
        if isinstance(bias, V):
            reads.append(bias.reg)
            b = bias.ap
        if isinstance(scale, V):
            reads.append(scale.reg)
            s = scale.ap
        writes = [out.reg]
        kw = {}
        if accum is not None:
            writes.append(accum.reg)
            kw["accum_out"] = accum.ap
        self.S.add("act", lambda e, o=out.ap, i=in_.ap, f=func, b=b, s=s, kw=kw:
                   e.activation(o, i, f, bias=b, scale=s, **kw), reads=reads, writes=writes)

    def tt(self, eng, out, in0, in1, op):
        self.S.add(eng, lambda e, o=out.ap, a=in0.ap, b=in1.ap, op=op: e.tensor_tensor(o, a, b, op),
                   reads=[in0.reg, in1.reg], writes=[out.reg])

    def ts(self, eng, out, in0, s1, s2, op0, op1=None, accum=None):
        reads = [in0.reg]
        a1, a2 = s1, s2
        if isinstance(s1, V):
            reads.append(s1.reg)
            a1 = s1.ap
        if isinstance(s2, V):
            reads.append(s2.reg)
            a2 = s2.ap
        writes = [out.reg]
        kw = {}
        if op1 is not None:
            kw["op1"] = op1
        if accum is not None:
            writes.append(accum.reg)
            kw["accum_out"] = accum.ap
        self.S.add(eng, lambda e, o=out.ap, i=in0.ap, a1=a1, a2=a2, op0=op0, kw=kw:
                   e.tensor_scalar(o, i, a1, a2, op0, **kw), reads=reads, writes=writes)

    def stt(self, out, in0, sc, in1, op0, op1):
        reads = [in0.reg, in1.reg]
        a = sc
        if isinstance(sc, V):
            reads.append(sc.reg)
            a = sc.ap
        self.S.add("dve", lambda e, o=out.ap, i0=in0.ap, a=a, i1=in1.ap, op0=op0, op1=op1:
                   e.scalar_tensor_tensor(o, i0, a, i1, op0, op1), reads=reads, writes=[out.reg])

    def copy(self, eng, out, in_):
        if eng == "act":
            self.S.add("act", lambda e, o=out.ap, i=in_.ap: e.copy(o, i), reads=[in_.reg], writes=[out.reg])
        else:
            self.S.add(eng, lambda e, o=out.ap, i=in_.ap: e.tensor_copy(o, i), reads=[in_.reg], writes=[out.reg])

    def recip(self, out, in_):
        self.S.add("dve", lambda e, o=out.ap, i=in_.ap: e.reciprocal(o, i), reads=[in_.reg], writes=[out.reg])

    def memset(self, eng, out, val):
        self.S.add(eng, lambda e, o=out.ap, v=val: e.memset(o, v), reads=[], writes=[out.reg])

    def dma(self, eng, out_ap, in_ap, reads, writes, slow=False):
        kw = {"allow_slow_non_contiguous": True} if slow else {}
        self.S.add(eng, lambda e, o=out_ap, i=in_ap, kw=kw: e.dma_start(out=o, in_=i, **kw),
                   reads=reads, writes=writes, dma=True)

    def evac(self, out, in_):
        self.rr ^= 1
        self.copy("act" if self.rr else "dve", out, in_)

    def psn(self, excl=()):
        i = self.psi
        while i in excl:
            i = (i + 1) % 8
        self.psi = (i + 1) % 8
        return i

    def pb(self, i, n=512, p=None):
        return self.PS[i].v((0, n), p=p)

    def build(self):
        nc = self.nc
        n_seq = self.n_seq
        ntok = n_seq * S
        dt = lambda name, shape, kind="ExternalInput", d=F32: nc.dram_tensor(name, shape, d, kind=kind).ap()
        self.x_d = dt("x", [ntok, D])
        self.out_d = dt("out", [ntok, D], kind="ExternalOutput")
        self.wd = {}
        for nm in ("ffn1", "ffn2"):
            self.wd[nm + "_g"] = dt(nm + "_w_gate", [2, D, DFF])
            self.wd[nm + "_u"] = dt(nm + "_w_up", [2, D, DFF])
            self.wd[nm + "_d"] = dt(nm + "_w_down", [2, DFF, D])
        self.wd["ab_in"] = dt("ab_w_in", [D, 1536])
        self.wd["pool_w"] = dt("pool_w", [4, 128, 128])
        self.wd["ab_out"] = dt("ab_w_out", [D, D])
        self.wd["c_in"] = dt("c_w_in", [D, 3656])
        self.wd["c_out"] = dt("c_w_out", [D, D])
        self.relb_d = dt("rel_bias", [32, 16])
        self.vecs_d = dt("vecs", [128, NVEC])
        self.consts_d = dt("consts", [128, NCONST])
        self.oh_d = dt("onehot", [32, FVL])
        self.fvh_d = dt("fvscrh", [16, FVL], kind="Internal", d=BF16)
        self.fvl_d = dt("fvscrl", [16, FVL], kind="Internal", d=BF16)
        self.bw_d = dt("bwscr", [16, 128, 1024], kind="Internal")
        self.ks_d = [dt(f"kscr{c}", [128, S], kind="Internal", d=BF16) for c in range(8)]
        self.vs_d = [dt(f"vscr{c}", [S, 128], kind="Internal", d=BF16) for c in range(8)]

        with ExitStack() as st:
            TOTW = 212000 // 4 // 8 * 8
            arena = st.enter_context(nc.sbuf_tensor("arena", [128, TOTW], F32))
            self.PS = []
            for i in range(8):
                p = st.enter_context(nc.psum_tensor(f"ps{i}", [128, 512], F32))
                self.PS.append(Buf(f"ps{i}", p, 0, (512,), F32))
                self.PS[-1].raw = p
            sems = {}
            for e in Sched.COMPUTE:
                sems[("c", e)] = st.enter_context(nc.semaphore(f"c_{e}"))
            for e in ("sp", "pool"):
                for k in range(Sched.KD):
                    sems[("d", e, k)] = st.enter_context(nc.semaphore(f"d_{e}{k}"))

            off = [0]

            def alloc(shape, dtype):
                b = Buf("sb", arena, off[0], shape, dtype)
                off[0] += (b.words + 7) // 8 * 8
                return b
            self.arena = arena
            self.consts = alloc((NCONST,), F32)
            self.vecs = alloc((NVEC,), F32)
            self.cbf = alloc((384,), BF16)
            self.relb = alloc((16,), F32)
            self.xT = alloc((8, CH), F32)
            self.hT = alloc((8, CH), BF16)
            self.ring = [alloc((4096,), BF16) for _ in range(4)]
            self.ki2 = alloc((S,), BF16)
            self.small = alloc((128,), F32)
            self.halo_u = alloc((4, 16), F32)
            self.halo_z = alloc((4, 32), F32)
            self.A0 = off[0]
            self.AW = TOTW - self.A0
            assert self.AW >= 26500, self.AW

            self.make_units()
            self.prologue()
            self.convert_all()
            nchunks = n_seq * (S // CH)
            self.wpos = 0
            self.wuse = 0
            self.load_x(0)
            for ci in range(nchunks):
                self.chunk(ci, nchunks)
            self.S.finalize()
            with nc.Block() as block:
                self.S.emit(nc, block, sems)
        return nc

    def ab(self, off_words, shape, dtype):
        return Buf("sb", self.arena, self.A0 + off_words, shape, dtype)

    def prologue(self):
        self.dma("sp", self.consts.ap, self.consts_d, [], [self.consts.v().reg])
        self.dma("sp", self.vecs.ap, self.vecs_d, [], [self.vecs.v().reg])
        self.dma("sp", self.relb.v(p=(0, 32)).ap, self.relb_d, [], [self.relb.v().reg])
        self.copy("dve", self.cbf.v((0, 128)), self.consts.v((C_ID, C_ID + 128)))
        self.copy("dve", self.cbf.v((128, 256)), self.consts.v((C_ONE, C_ONE + 128)))
        self.copy("dve", self.cbf.v((256, 384)), self.consts.v((C_J, C_J + 128)))
        self.Jb = self.cbf.v((256, 384))
        self.ident = self.consts.v((C_ID, C_ID + 128))
        self.identb = self.cbf.v((0, 128))
        self.onesb = self.cbf.v((128, 256))
        self.onesf = self.consts.v((C_ONE, C_ONE + 128))
        if self.stages >= 2:
            um = {u["name"]: u for u in self.units}
            for j in range(4):
                dg = self.ab((j % 2) * 2048, (31, 128), BF16)
                for tap in range(31):
                    self.ts("dve", dg.v(tap), self.identb,
                            self.vecs.v((V_CW + j * 31 + tap, V_CW + j * 31 + tap + 1)), None, ALU.mult)
                self.dma("sp", um[f"cdiag{j}"]["dram"], dg.ap.rearrange("p a b -> p (a b)"), [dg.v().reg],
                         [(f"w_cdiag{j}", 0, 1)])
        if self.stages >= 5:
            oh = self.ab(0, (FVL,), F32)
            fvs = self.ab(1160, (FVL,), F32)
            ohb = self.ab(2320, (FVL,), BF16)
            fvh = self.ab(2900, (FVL,), BF16)
            fvf = self.ab(3480, (FVL,), F32)
            fvl = self.ab(4640, (FVL,), BF16)
            rbh = self.ab(5220, (16,), BF16)
            rbf = self.ab(5230, (16,), F32)
            rbl = self.ab(5250, (16,), BF16)
            P32 = (0, 32)
            P16 = (0, 16)
            self.dma("sp", oh.v(p=P32).ap, self.oh_d, [], [oh.v().reg])
            self.copy("dve", ohb.v(p=P32), oh.v(p=P32))
            self.copy("dve", rbh.v(p=P32), self.relb.v(p=P32))
            self.copy("dve", rbf.v(p=P32), rbh.v(p=P32))
            self.tt("dve", rbl.v(p=P32), self.relb.v(p=P32), rbf.v(p=P32), ALU.subtract)
            for b0 in range(0, FVL, 512):
                w = min(512, FVL - b0)
                pi = self.psn()
                self.mm(self.pb(pi, w, p=P16), rbh.v(p=P32), ohb.v((b0, b0 + w), p=P32), True, False)
                self.mm(self.pb(pi, w, p=P16), rbl.v(p=P32), ohb.v((b0, b0 + w), p=P32), False, True)
                self.copy("dve", fvs.v((b0, b0 + w), p=P16), self.pb(pi, w, p=P16))
            self.copy("dve", fvh.v(p=P16), fvs.v(p=P16))
            self.copy("dve", fvf.v(p=P16), fvh.v(p=P16))
            self.tt("dve", fvl.v(p=P16), fvs.v(p=P16), fvf.v(p=P16), ALU.subtract)
            self.dma("sp", self.fvh_d, fvh.v(p=P16).ap, [fvh.v().reg], [("fvh", 0, 1)])
            self.dma("sp", self.fvl_d, fvl.v(p=P16).ap, [fvl.v().reg], [("fvl", 0, 1)])
            for head in range(16):
                Wh = self.ab(6000 + (head % 2) * 1024, (1024,), BF16)
                Wl = self.ab(6000 + (head % 2) * 1024 + 512, (1024,), BF16)
                bwt = self.ab(8048 + (head % 2) * 1024, (1024,), F32)
                for (Wx, fd, nm) in ((Wh, self.fvh_d, "fvh"), (Wl, self.fvl_d, "fvl")):
                    src = bass.AP(tensor=fd.tensor, offset=head * FVL, ap=[[1, 128], [1, 1024]])
                    self.dma("sp", Wx.ap, src, [(nm, 0, 1)], [Wx.v().reg])
                for hf in range(2):
                    pi = self.psn()
                    self.mm(self.pb(pi), self.Jb, Wh.v((hf * 512, hf * 512 + 512)), True, False)
                    self.mm(self.pb(pi), self.Jb, Wl.v((hf * 512, hf * 512 + 512)), False, True)
                    self.evac(bwt.v((hf * 512, hf * 512 + 512)), self.pb(pi))
                self.dma("sp", self.bw_d[head], bwt.ap, [bwt.v().reg], [("bw", head, head + 1)])

    def make_units(self):
        U = []
        wd = self.wd

        def colunit(name, mat, cols):
            tot = sum(n for _, n in cols)
            parts = []
            o = 0
            src = mat.rearrange("(k p) f -> p k f", p=128)
            for lo, n in cols:
                parts.append((o, n, src[:, :, lo:lo + n]))
                o += n
            U.append(dict(name=name, size=8 * tot, kind="col", tot=tot, parts=parts))

        def ffn(l, nm):
            for u in range(11):
                parts = []
                for gi, key in enumerate(("_g", "_u")):
                    src = wd[nm + key][l].rearrange("(k p) f -> p k f", p=128)[:, :, 256 * u:256 * u + 256]
                    parts.append((gi * 2048, 256, src))
                U.append(dict(name=f"{nm}{l}gu{u}", size=4096, kind="gu", parts=parts))
            for c in range(8):
                src = wd[nm + "_d"][l].rearrange("(f p) d -> p f d", p=128)[:, :, 128 * c:128 * c + 128]
                U.append(dict(name=f"{nm}{l}d{c}", size=NF * 128, kind="dn", parts=[(0, 128, src)]))
        st = self.stages
        if st >= 1:
            ffn(0, "ffn1")
        if st >= 2:
            colunit("abin0", wd["ab_in"], [(0, 512)])
            colunit("abin1", wd["ab_in"], [(512, 256), (1024, 256)])
            colunit("abin2", wd["ab_in"], [(768, 256), (1280, 256)])
            U.append(dict(name="poolw", size=512, kind="pw",
                          parts=[(0, 128, wd["pool_w"].rearrange("g c d -> c g d"))]))
            for j in range(4):
                U.append(dict(name=f"cdiag{j}", size=3968, kind="pre", parts=[]))
            colunit("about0", wd["ab_out"], [(0, 512)])
            colunit("about1", wd["ab_out"], [(512, 512)])
        if st >= 3:
            ffn(0, "ffn2")
        if st >= 4:
            ffn(1, "ffn1")
        if st >= 5:
            colunit("cqi", wd["c_in"], [(3072, 512)])
            colunit("ckw", wd["c_in"], [(3584, 64), (3584, 64), (3648, 8)])
            for c in range(8):
                colunit(f"cqkv{c}", wd["c_in"], [(128 * c, 128), (1024 + 128 * c, 128), (2048 + 128 * c, 128)])
            colunit("cout0", wd["c_out"], [(0, 512)])
            colunit("cout1", wd["c_out"], [(512, 512)])
        if st >= 6:
            ffn(1, "ffn2")
        self.units = U
        for i, u in enumerate(U):
            u["dram"] = self.nc.dram_tensor("w_" + u["name"], [128, u["size"]], BF16, kind="Internal").ap()
            u["idx"] = i

    def convert_all(self):
        for u in self.units:
            d = u["dram"]
            reg = ("w_" + u["name"], 0, 1)
            if u["kind"] == "col":
                dv = d.rearrange("p (k f) -> p k f", k=8)
                for (o, n, src) in u["parts"]:
                    self.dma("pool", dv[:, :, o:o + n], src, [], [reg])
            elif u["kind"] == "gu":
                for (o, n, src) in u["parts"]:
                    self.dma("pool", d[:, o:o + 2048].rearrange("p (k f) -> p k f", k=8), src, [], [reg])
            elif u["kind"] == "pre":
                pass
            elif u["kind"] == "dn":
                self.dma("pool", d.rearrange("p (f d) -> p f d", f=NF), u["parts"][0][2], [], [reg])
            else:
                self.dma("pool", d.rearrange("p (g d) -> p g d", g=4), u["parts"][0][2], [], [reg])

    def wload_upto(self, gidx):
        nu = len(self.units)
        total = nu * self.n_seq * (S // CH)
        while self.wpos <= gidx and self.wpos < total:
            u = self.units[self.wpos % nu]
            slot = self.ring[self.wpos % 4]
            self.dma("sp", slot.v((0, u["size"])).ap, u["dram"], [("w_" + u["name"], 0, 1)],
                     [slot.v((0, u["size"])).reg])
            self.wpos += 1

    def wget(self, name):
        nu = len(self.units)
        g = self.wuse
        u = self.units[g % nu]
        while u["name"] != name and "DBG_CUT" in os.environ:
            self.wload_upto(g + 3)
            self.wuse += 1
            g = self.wuse
            u = self.units[g % nu]
        assert u["name"] == name, (u["name"], name)
        self.wload_upto(g + 3)
        self.wuse += 1
        return self.ring[g % 4]

    def stage_buf(self):
        return self.ab(self.AW - 8192 - 8, (8, D), F32)

    def load_x(self, ci):
        stg = self.stage_buf()
        r0 = ci * CH
        self.dma("sp", stg.ap, self.x_d[r0:r0 + CH, :].rearrange("(t p) d -> p t d", p=128),
                 [], [stg.v().reg])

    def transpose_in(self):
        stg = self.stage_buf()
        for k in range(8):
            for tb in range(2):
                pi = self.psn()
                for j in range(4):
                    self.tr(self.PS[pi].v((j * 128, j * 128 + 128)),
                            stg.v(tb * 4 + j, (k * 128, k * 128 + 128)), self.ident)
                self.evac(self.xT.v(k, (tb * 512, tb * 512 + 512)), self.pb(pi))

    def gcol(self, vi, k):
        return self.vecs.v((V_G + vi * 8 + k, V_G + vi * 8 + k + 1))

    def norm_stats(self, n, sqoff):
        sq = self.ab(sqoff, (8, 512), BF16)
        sd = self.ab(sqoff + 2048, (512,), F32)
        rstd = self.ab(sqoff + 2048 + 512, (512,), F32)
        self.act(sq.v(), self.xT.v(None, (n * 512, n * 512 + 512)), AF.Square)
        pi = self.psn()
        for k in range(8):
            self.mm(self.pb(pi), self.onesb, sq.v(k), k == 0, k == 7)
        self.act(sd.v(), self.pb(pi), AF.Sqrt, bias=self.epsb, scale=1.0 / D)
        self.recip(rstd.v(), sd.v())
        return rstd

    def rmsnorm(self, vi, sqoff):
        for n in range(NTT):
            rstd = self.norm_stats(n, sqoff)
            for k in range(8):
                self.stt(self.hT.v(k, (n * 512, n * 512 + 512)), self.xT.v(k, (n * 512, n * 512 + 512)),
                         self.gcol(vi, k), rstd.v(), ALU.mult, ALU.mult)

    def final_out(self, ci):
        ost = self.ab(11264, (8, D), F32)
        yb = [self.ab(i * 512, (512,), F32) for i in range(2)]
        sqoff = 1024
        for n in range(NTT):
            if self.do_final:
                rstd = self.norm_stats(n, sqoff)
            for k in range(8):
                y = yb[k % 2]
                xs = self.xT.v(k, (n * 512, n * 512 + 512))
                if self.do_final:
                    self.stt(y.v(), xs, self.gcol(6, k), rstd.v(), ALU.mult, ALU.mult)
                else:
                    self.copy("dve", y.v(), xs)
                pi = self.psn()
                for j in range(4):
                    self.tr(self.PS[pi].v((j * 128, j * 128 + 128)), y.v((j * 128, j * 128 + 128)), self.ident)
                pv = V(self.PS[pi].ap.rearrange("p (a b) -> p a b", a=4), self.PS[pi].v().reg)
                self.evac(ost.v((n * 4, n * 4 + 4), (k * 128, k * 128 + 128)), pv)
        r0 = ci * CH
        self.dma("sp", self.out_d[r0:r0 + CH, :].rearrange("(t p) d -> p t d", p=128), ost.ap,
                 [ost.v().reg], [("out", r0, r0 + CH)])

    def ffn(self, l, nm, vi):
        aT = self.ab(0, (NF, CH), BF16)
        sg = [self.ab(11264 + i * 256, (512,), BF16) for i in range(2)]
        self.rmsnorm(vi, 11264 + 512)
        for u in range(11):
            wt = self.wget(f"{nm}{l}gu{u}")
            for f2 in range(2):
                f = 2 * u + f2
                for n in range(NTT):
                    pg = self.psn()
                    pu = self.psn()
                    hs = lambda k: self.hT.v(k, (n * 512, n * 512 + 512))
                    for k in range(8):
                        o = k * 256 + f2 * 128
                        self.mm(self.pb(pg), wt.v((o, o + 128)), hs(k), k == 0, k == 7)
                    for k in range(8):
                        o = 2048 + k * 256 + f2 * 128
                        self.mm(self.pb(pu), wt.v((o, o + 128)), hs(k), k == 0, k == 7)
                    s = sg[(f * NTT + n) % 2]
                    self.act(s.v(), self.pb(pg), AF.Silu)
                    self.tt("dve", aT.v(f, (n * 512, n * 512 + 512)), s.v(), self.pb(pu), ALU.mult)
        for c in range(8):
            wt = self.wget(f"{nm}{l}d{c}")
            for n in range(NTT):
                po = self.psn()
                for f in range(NF):
                    self.mm(self.pb(po), wt.v((f * 128, f * 128 + 128)), aT.v(f, (n * 512, n * 512 + 512)),
                            f == 0, f == NF - 1)
                xs = self.xT.v(c, (n * 512, n * 512 + 512))
                self.stt(xs, self.pb(po), 0.5, xs, ALU.mult, ALU.add)

    def chunk(self, ci, nchunks):
        h = ci % (S // CH)
        self.transpose_in()
        st = self.stages
        last = None
        phases = []
        if st >= 1:
            phases.append(lambda: self.ffn(0, "ffn1", 0))
        if st >= 2:
            phases.append(lambda: self.mixer0(h))
        if st >= 3:
            phases.append(lambda: self.ffn(0, "ffn2", 2))
        if st >= 4:
            phases.append(lambda: self.ffn(1, "ffn1", 3))
        if st >= 5:
            phases.append(lambda: self.attn(h))
        if st >= 6:
            phases.append(lambda: self.ffn(1, "ffn2", 5))
        pref = st in (1, 3, 4, 6)
        for i, ph in enumerate(phases):
            if pref and i == len(phases) - 1 and ci + 1 < nchunks:
                self.load_x(ci + 1)
            ph()
        self.final_out(ci)
        if not pref and ci + 1 < nchunks:
            self.load_x(ci + 1)

    def mixer0(self, h):
        up = self.ab(0, (4, 1040), F32)
        tA = self.ab(4160, (1040,), F32)
        tB = self.ab(5200, (1040,), F32)
        zc = self.ab(0, (4, 1024), F32)
        z = self.ab(6240, (4, 1056), BF16)
        dif = self.ab(10464, (1024,), BF16)
        sgm = [self.ab(10976 + i * 512, (512,), F32) for i in range(2)]
        sqoff = 12000
        sqc = self.ab(12000, (4, 512), F32)
        sdl = self.ab(12000 + 2048, (512,), F32)
        rsl = self.ab(12000 + 2560, (512,), F32)
        yab = self.hT
        T = lambda n: (n * 512, n * 512 + 512)
        self.rmsnorm(1, sqoff)
        if h == 0:
            self.memset("dve", up.v(None, (0, 16)), 0.0)
            self.memset("dve", z.v(None, (0, 32)), 0.0)
        else:
            self.copy("dve", up.v(None, (0, 16)), self.halo_u.v())
            self.copy("dve", z.v(None, (0, 32)), self.halo_z.v())
        wt = self.wget("abin0")
        for g in range(4):
            for n in range(NTT):
                pi = self.psn()
                for k in range(8):
                    o = k * 512 + g * 128
                    self.mm(self.pb(pi), wt.v((o, o + 128)), self.hT.v(k, T(n)), k == 0, k == 7)
                self.evac(up.v(g, (16 + n * 512, 16 + n * 512 + 512)), self.pb(pi))
        for ui in range(2):
            wt = self.wget(f"abin{ui + 1}")
            for jj in range(2):
                j = 2 * ui + jj
                for n in range(NTT):
                    pv = self.psn()
                    pg = self.psn()
                    for k in range(8):
                        o = k * 512 + jj * 128
                        self.mm(self.pb(pv), wt.v((o, o + 128)), self.hT.v(k, T(n)), k == 0, k == 7)
                    for k in range(8):
                        o = k * 512 + 256 + jj * 128
                        self.mm(self.pb(pg), wt.v((o, o + 128)), self.hT.v(k, T(n)), k == 0, k == 7)
                    sg = sgm[(j * NTT + n) % 2]
                    self.act(sg.v(), self.pb(pg), AF.Sigmoid)
                    self.tt("dve", z.v(j, (32 + n * 512, 32 + n * 512 + 512)), sg.v(), self.pb(pv), ALU.mult)
        CUT = int(os.environ.get("DBG_CUT", "99"))
        if CUT <= 3:
            return
        if h == 0:
            self.copy("dve", self.halo_u.v(), up.v(None, (1024, 1040)))
            self.copy("dve", self.halo_z.v(), z.v(None, (1024, 1056)))
        pw = self.wget("poolw")
        for g in range(4):
            w = 2 ** (g + 1)
            src = None
            bufs = [tA, tB]
            for lv in range(1, g + 2):
                sh = 2 ** (lv - 1)
                lo = 2 ** lv
                dst = bufs[lv % 2]
                if lv == 1:
                    self.tt("dve", dst.v((lo, 1040)), up.v(g, (lo, 1040)), up.v(g, (lo - sh, 1040 - sh)), ALU.add)
                else:
                    self.tt("dve", dst.v((lo, 1040)), src.v((lo, 1040)), src.v((lo - sh, 1040 - sh)), ALU.add)
                src = dst
            self.stt(dif.v(), src.v((16, 1040)), 1.0 / w, up.v(g, (16, 1040)), ALU.mult, ALU.subtract)
            if h == 0:
                oth = bufs[(g + 2) % 2]
                self.tt("dve", oth.v((0, w - 1)), src.v((16, 16 + w - 1)),
                        self.consts.v((C_IC, C_IC + w - 1)), ALU.mult)
                self.tt("dve", dif.v((0, w - 1)), oth.v((0, w - 1)), up.v(g, (16, 16 + w - 1)), ALU.subtract)
            for n in range(NTT):
                pi = self.psn()
                self.mm(self.pb(pi), pw.v((g * 128, g * 128 + 128)), dif.v(T(n)), True, True)
                self.ts("dve", yab.v(g, T(n)), self.pb(pi), self.vecs.v((V_PS + g, V_PS + g + 1)), None, ALU.mult)
        if CUT <= 4:
            return
        for j in range(4):
            wt = self.wget(f"cdiag{j}")
            cb = self.vecs.v((V_CB + j, V_CB + j + 1))
            for n in range(NTT):
                pi = self.psn()
                for tap in range(31):
                    o = 2 + tap + n * 512
                    self.mm(self.pb(pi), wt.v((tap * 128, tap * 128 + 128)), z.v(j, (o, o + 512)), tap == 0, tap == 30)
                self.act(zc.v(j, T(n)), self.pb(pi), AF.Identity, bias=cb)
        if CUT <= 5:
            return
        sqb = self.ab(12000, (4, 512), BF16)
        zcb = self.ab(12000 + 1024, (4, 512), BF16)
        for n in range(NTT):
            pm = self.psn()
            self.copy("act", zcb.v(), zc.v(None, T(n)))
            for j in range(4):
                self.mm(self.pb(pm), self.onesb, zcb.v(j), j == 0, j == 3)
            for j in range(4):
                self.stt(zc.v(j, T(n)), self.pb(pm), -1.0 / 512, zc.v(j, T(n)), ALU.mult, ALU.add)
            self.act(sqb.v(), zc.v(None, T(n)), AF.Square)
            pv = self.psn()
            for j in range(4):
                self.mm(self.pb(pv), self.onesb, sqb.v(j), j == 0, j == 3)
            self.act(sdl.v(), self.pb(pv), AF.Sqrt, bias=self.epsb, scale=1.0 / 512)
            self.recip(rsl.v(), sdl.v())
            for j in range(4):
                self.tt("dve", zc.v(j, T(n)), zc.v(j, T(n)), rsl.v(), ALU.mult)
                self.act(yab.v(4 + j, T(n)), zc.v(j, T(n)), AF.Silu,
                         bias=self.vecs.v((V_LB + j, V_LB + j + 1)), scale=self.vecs.v((V_LG + j, V_LG + j + 1)))
        if CUT <= 6:
            return
        for ui in range(2):
            wt = self.wget(f"about{ui}")
            for cc in range(4):
                c = ui * 4 + cc
                for n in range(NTT):
                    pi = self.psn()
                    for k in range(8):
                        o = k * 512 + cc * 128
                        self.mm(self.pb(pi), wt.v((o, o + 128)), yab.v(k, T(n)), k == 0, k == 7)
                    self.tt("dve", self.xT.v(c, T(n)), self.xT.v(c, T(n)), self.pb(pi), ALU.add)

    def attn(self, h):
        T0 = CH * h
        ab = self.ab
        qT = ab(0, (8, CH), BF16)
        qiT = ab(4096, (4, CH), BF16)
        maskT = ab(6144, (16, 512), BF16)
        sc = ab(10240, (S,), F32)
        mrow = ab(12288, (S,), BF16)
        kbuf = [ab(13312 + i * 1024, (S,), BF16) for i in range(2)]
        vbuf = [ab(15360 + i * 1024, (16, 128), BF16) for i in range(2)]
        kst = [ab(17408 + i * 512, (CH,), BF16) for i in range(2)]
        vst = [ab(18432 + i * 512, (8, 128), BF16) for i in range(2)]
        Whs = [ab(19456, (1024,), BF16), ab(25216, (1024,), BF16)]
        Wls = [ab(19456 + 512, (1024,), BF16), ab(25216 + 512, (1024,), BF16)]
        biasWs = [ab(20480, (1024,), F32), ab(26240, (1024,), F32)]
        pT = [ab(21504 + i * 256, (512,), BF16) for i in range(4)]
        tmpf = [ab(22528 + i * 512, (512,), F32) for i in range(2)]
        rbuf = [ab(23552 + i * 512, (512,), F32) for i in range(2)]
        rdb = ab(24576, (512,), F32)
        wts = ab(25088, (64,), F32)
        sm = ab(25152, (64,), F32)
        sqoff = 6144
        attnT = self.hT
        A = sm.v((0, 1))
        thr = [sm.v((1, 2)), sm.v((2, 3))]
        cnt = sm.v((3, 4))
        gg = sm.v((4, 5))
        thrneg = sm.v((5, 6))
        stp = Buf("sb", self.arena, self.A0 + 25152 + 8, (NIT,), F32)
        st2 = Buf("sb", self.arena, self.A0 + 25152 + 8 + NIT, (NIT,), F32)
        T = lambda n: (n * 512, n * 512 + 512)
        self.rmsnorm(4, sqoff)
        self.memset("dve", thrneg, -1.0e29)
        wt = self.wget("cqi")
        for c4 in range(4):
            for n in range(NTT):
                pi = self.psn()
                for k in range(8):
                    o = k * 512 + c4 * 128
                    self.mm(self.pb(pi), wt.v((o, o + 128)), self.hT.v(k, T(n)), k == 0, k == 7)
                self.evac(qiT.v(c4, T(n)), self.pb(pi))
        wt = self.wget("ckw")
        for n in range(NTT):
            pi = self.psn()
            for k in range(8):
                self.mm(self.pb(pi), wt.v((k * 136, k * 136 + 128)), self.hT.v(k, T(n)), k == 0, k == 7)
            self.evac(self.ki2.v((T0 + n * 512, T0 + n * 512 + 512)), self.pb(pi))
        pi = self.psn()
        for tt in range(8):
            for k in range(8):
                self.mm(self.PS[pi].v((tt * 8, tt * 8 + 8)), self.hT.v(k, (tt * 128, tt * 128 + 128)),
                        wt.v((k * 136 + 128, k * 136 + 136)), k == 0, k == 7)
        self.copy("dve", wts.v(), self.PS[pi].v((0, 64)))
        wabs = ab(27264, (64,), F32)
        wsg = ab(27264 + 64, (64,), F32)
        dgs = ab(27264 + 128, (8, 128), BF16)
        rb = [ab(23552 + i * 256, (512,), BF16) for i in range(4)]
        dgs2 = ab(27264 + 128 + 512, (8, 128), BF16)
        smB = ab(27264 + 128 + 1024, (64,), F32)
        sc2 = ab(13312, (S,), F32)
        mrow2 = ab(15360, (S,), BF16)
        self.act(wsg.v(), wts.v(), AF.Sign)
        self.tt("dve", wabs.v(), wts.v(), wsg.v(), ALU.mult)
        for c in range(8):
            wt = self.wget(f"cqkv{c}")
            for n in range(NTT):
                pi = self.psn()
                for k in range(8):
                    self.mm(self.pb(pi), wt.v((k * 384, k * 384 + 128)), self.hT.v(k, T(n)), k == 0, k == 7)
                self.evac(qT.v(c, T(n)), self.pb(pi))
            ks = kst[c % 2]
            for n in range(NTT):
                pi = self.psn()
                for k in range(8):
                    self.mm(self.pb(pi), wt.v((k * 384 + 128, k * 384 + 256)), self.hT.v(k, T(n)), k == 0, k == 7)
                self.evac(ks.v(T(n)), self.pb(pi))
            self.dma("sp", self.ks_d[c][:, T0:T0 + CH], ks.ap, [ks.v().reg], [(f"kscr{c}", T0, T0 + CH)])
            vs = vst[c % 2]
            for hf in range(2):
                pi = self.psn()
                for j4 in range(4):
                    tt = hf * 4 + j4
                    for k in range(8):
                        self.mm(self.PS[pi].v((j4 * 128, j4 * 128 + 128)), self.hT.v(k, (tt * 128, tt * 128 + 128)),
                                wt.v((k * 384 + 256, k * 384 + 384)), k == 0, k == 7)
                pv = V(self.PS[pi].ap.rearrange("p (a b) -> p a b", a=4), self.PS[pi].v().reg)
                self.evac(vs.v((hf * 4, hf * 4 + 4)), pv)
            self.dma("sp", self.vs_d[c][T0:T0 + CH, :].rearrange("(j p) d -> p j d", p=128), vs.ap,
                     [vs.v().reg], [(f"vscr{c}", T0, T0 + CH)])
        for tb in range(2):
            B = 2 * h + tb
            for t4p in (0, 2):
                tiles = []
                for ti in range(2):
                    t4 = t4p + ti
                    tt = tb * 4 + t4
                    I = 8 * h + tt
                    L = 128 * (I + 1)
                    scb = sc if ti == 0 else sc2
                    mr = mrow if ti == 0 else mrow2
                    smx = sm if ti == 0 else smB
                    A_ = smx.v((0, 1))
                    thr_ = [smx.v((1, 2)), smx.v((2, 3))]
                    cnt_ = smx.v((3, 4))
                    gg_ = smx.v((4, 5))
                    stp_ = Buf("sb", self.arena, smx.off + 8, (NIT,), F32)
                    st2_ = Buf("sb", self.arena, smx.off + 8 + NIT, (NIT,), F32)
                    tiles.append((t4, tt, I, L, scb, mr, A_, thr_, cnt_, gg_, stp_, st2_))
                    nblk = (L + 511) // 512
                    dg = dgs if ti == 0 else dgs2
                    for hh in range(8):
                        self.ts("dve", dg.v(hh), self.identb, wsg.v((tt * 8 + hh, tt * 8 + hh + 1)), None, ALU.mult)
                    for sb in range(nblk):
                        wd_ = min(512, L - 512 * sb)
                        scs = scb.v((sb * 512, sb * 512 + wd_))
                        pacc = 4 + (sb % 2)

                        def zq(hh):
                            pr = (64 * (hh % 2), 64 * (hh % 2) + 64)
                            self.mm(self.pb(hh % 4, wd_), qiT.v(hh // 2, (tt * 128, tt * 128 + 128), p=pr),
                                    self.ki2.v((sb * 512, sb * 512 + wd_), p=pr), True, True)
                        zq(0)
                        zq(1)
                        for hh in range(8):
                            r = rb[hh % 4]
                            self.act(r.v((0, wd_)), self.pb(hh % 4, wd_), AF.Relu,
                                     scale=wabs.v((tt * 8 + hh, tt * 8 + hh + 1)))
                            if hh + 2 < 8:
                                zq(hh + 2)
                            self.mm(self.pb(pacc, wd_), dg.v(hh), r.v((0, wd_)), hh == 0, hh == 7)
                        self.copy("dve", scs, self.pb(pacc, wd_))
                    if I >= 2:
                        self.S.add("dve", lambda e, o=A_.ap, i=scb.v((0, L)).ap:
                                   e.tensor_reduce(o, i, AX.X, ALU.max, apply_absolute_value=True),
                                   reads=[scb.v((0, L)).reg], writes=[A_.reg])
                    self.tt("dve", scb.v((L - 128, L)), scb.v((L - 128, L)), self.consts.v((C_CM, C_CM + 128)), ALU.add)
                    if I >= 2:
                        self.ts("dve", stp_.v(), self.consts.v((C_P2, C_P2 + NIT)), A_, None, ALU.mult)
                        self.ts("dve", st2_.v(), stp_.v(), 2.0, None, ALU.mult)
                        self.memset("dve", thr_[0], 0.0)
                for k in range(NIT):
                    for (t4, tt, I, L, scb, mr, A_, thr_, cnt_, gg_, stp_, st2_) in tiles:
                        if I < 2:
                            continue
                        cur, nxt = thr_[k % 2], thr_[(k + 1) % 2]
                        self.ts("dve", mr.v((0, L)), scb.v((0, L)), cur, None, ALU.is_ge, ALU.add, accum=cnt_)
                        self.ts("dve", gg_, cnt_, 255.5, st2_.v((k, k + 1)), ALU.is_ge, ALU.mult)
                        self.stt(nxt, gg_, stp_.v((k, k + 1)), cur, ALU.subtract, ALU.add)
                for (t4, tt, I, L, scb, mr, A_, thr_, cnt_, gg_, stp_, st2_) in tiles:
                    thf = thr_[NIT % 2] if I >= 2 else thrneg
                    self.ts("dve", mr.v((0, L)), scb.v((0, L)), thf, None, ALU.is_ge)
                    for j0 in range(0, I + 1, 4):
                        nj = min(4, I + 1 - j0)
                        pi = 6 + ((j0 // 4) % 2)
                        psb = self.PS[pi].ap.bitcast(BF16)
                        preg = self.PS[pi].v().reg
                        for jj in range(nj):
                            self.tr(V(psb[:, jj * 128:jj * 128 + 128], preg),
                                    mr.v(((j0 + jj) * 128, (j0 + jj) * 128 + 128)), self.identb)
                        src = V(psb[:, 0:nj * 128].rearrange("p (a b) -> p a b", a=nj), preg)
                        self.evac(maskT.v((j0, j0 + nj), (t4 * 128, t4 * 128 + 128)), src)
                    if I + 1 < 4 * B + 4:
                        self.memset("pool", maskT.v((I + 1, 4 * B + 4), (t4 * 128, t4 * 128 + 128)), 0.0)
            nj_all = 4 * B + 4
            Lb = 128 * nj_all
            def bias_build(head):
                bW = biasWs[head % 2]
                self.dma("sp", bW.ap, self.bw_d[head], [("bw", head, head + 1)], [bW.v().reg])

            def kv_load(c):
                kb = kbuf[c % 2]
                vb = vbuf[c % 2]
                self.dma("sp", kb.v((0, Lb)).ap, self.ks_d[c][:, 0:Lb], [(f"kscr{c}", 0, Lb)], [kb.v((0, Lb)).reg])
                self.dma("sp", vb.v((0, nj_all)).ap, self.vs_d[c][0:Lb, :].rearrange("(j p) d -> p j d", p=128),
                         [(f"vscr{c}", 0, Lb)], [vb.v((0, nj_all)).reg])

            LA = 3
            items = [(c, hd, j) for c in range(8) for hd in range(2) for j in range(nj_all)]
            PR = lambda hd: (64 * hd, 64 * hd + 64)

            def qk(idx):
                c, hd, j = items[idx]
                self.mm(self.pb(4 + idx % 4), kbuf[c % 2].v((j * 128, j * 128 + 128), p=PR(hd)),
                        qT.v(c, T(tb), p=PR(hd)), True, True)
            kv_load(0)
            bias_build(0)
            for idx in range(min(LA, len(items))):
                qk(idx)
            for idx, (c, hd, j) in enumerate(items):
                head = 2 * c + hd
                pr = PR(hd)
                vb = vbuf[c % 2]
                biasW = biasWs[head % 2]
                po, pd = (0, 1) if head % 2 == 0 else (2, 3)
                if j == 0:
                    if hd == 0 and c + 1 < 8:
                        kv_load(c + 1)
                    if head + 1 < 16:
                        bias_build(head + 1)
                pS = 4 + idx % 4
                r = j - 4 * B
                pt = pT[idx % 4]
                if r >= -1:
                    tf = tmpf[idx % 2]
                    o = 384 - 128 * r
                    self.stt(tf.v(), self.pb(pS), 0.125, biasW.v((o, o + 512)), ALU.mult, ALU.add)
                    self.act(pt.v(), tf.v(), AF.Exp)
                else:
                    self.act(pt.v(), self.pb(pS), AF.Exp,
                             bias=self.vecs.v((V_B31 + head, V_B31 + head + 1)), scale=0.125)
                self.tt("pool" if idx % 2 else "dve", pt.v(), pt.v(), maskT.v(j), ALU.mult)
                if idx + LA < len(items):
                    qk(idx + LA)
                self.mm(self.pb(po), vb.v(j), pt.v(), j == 0, j == nj_all - 1)
                self.mm(self.pb(pd), self.onesb, pt.v(), j == 0, j == nj_all - 1)
                if j == nj_all - 1:
                    self.recip(rdb.v(p=pr), self.pb(pd, p=pr))
                    self.tt("dve", attnT.v(c, T(tb), p=pr), self.pb(po, p=pr), rdb.v(p=pr), ALU.mult)
        for ui in range(2):
            wt = self.wget(f"cout{ui}")
            for cc in range(4):
                c = ui * 4 + cc
                for n in range(NTT):
                    pi = self.psn()
                    for k in range(8):
                        o = k * 512 + cc * 128
                        self.mm(self.pb(pi), wt.v((o, o + 128)), attnT.v(k, T(n)), k == 0, k == 7)
                    self.tt("dve", self.xT.v(c, T(n)), self.xT.v(c, T(n)), self.pb(pi), ALU.add)


def _eps_setup(P):
    P.epsb = P.small.v((0, 1))
    P.memset("dve", P.epsb, EPS)


_orig_prologue = Prog.prologue


def _prologue(self):
    _eps_setup(self)
    _orig_prologue(self)


Prog.prologue = _prologue


def t5_bucket_np(dist):
    max_exact = 16
    d = np.maximum(dist, max_exact).astype(np.float32)
    large = max_exact + (np.log(d / max_exact) / math.log(128 / max_exact) * 16).astype(np.int32)
    large = np.minimum(large, 31)
    return np.where(dist < max_exact, dist, large)


def host_consts():
    c = np.zeros((128, NCONST), np.float32)
    c[:, C_ID:C_ID + 128] = np.eye(128, dtype=np.float32)
    c[:, C_J:C_J + 128] = np.eye(128, dtype=np.float32)[::-1]
    t = np.arange(128)[:, None]
    s = np.arange(128)[None, :]
    c[:, C_CM:C_CM + 128] = np.where(s <= t, 0.0, NEG)
    c[:, C_ONE:C_ONE + 128] = 1.0
    c[:, C_P2:C_P2 + NIT] = (0.5 ** np.arange(1, NIT + 1))[None, :]
    c[:, C_IC:C_IC + 16] = (1.0 / np.arange(1, 17))[None, :]
    oh = np.zeros((32, FVL), np.float32)
    dd = np.arange(FVL) - 511
    ok = dd >= 0
    b = t5_bucket_np(np.maximum(dd, 0))
    oh[b[ok], np.arange(FVL)[ok]] = 1.0
    return c, oh


def host_vecs(inp):
    v = np.zeros((128, NVEC), np.float32)
    gl = [inp["ffn1_norm"][0], inp["mix_norm"][0], inp["ffn2_norm"][0],
          inp["ffn1_norm"][1], inp["mix_norm"][1], inp["ffn2_norm"][1], inp["final_norm"]]
    for i, g in enumerate(gl):
        v[:, V_G + i * 8:V_G + i * 8 + 8] = np.asarray(g, np.float32).reshape(8, 128).T
    v[:, V_PS:V_PS + 4] = np.asarray(inp["pool_scale"][0], np.float32).reshape(4, 128).T
    cw = np.asarray(inp["conv_w"][0], np.float32)
    v[:, V_CW:V_CW + 124] = cw.reshape(31, 4, 128).transpose(2, 1, 0).reshape(128, 124)
    v[:, V_CB:V_CB + 4] = np.asarray(inp["conv_b"][0], np.float32).reshape(4, 128).T
    v[:, V_LG:V_LG + 4] = np.asarray(inp["conv_ln_g"][0], np.float32).reshape(4, 128).T
    v[:, V_LB:V_LB + 4] = np.asarray(inp["conv_ln_b"][0], np.float32).reshape(4, 128).T
    v[:, V_B31:V_B31 + 16] = np.asarray(inp["rel_bias"], np.float32)[31][None, :]
    return v


_CACHE = {}


def run(inputs, n_seq=4, stages=6, do_final=True, n_cores=8, trace=False):
    key = (n_seq, stages, do_final)
    if key not in _CACHE:
        _CACHE[key] = Prog(n_seq, stages, do_final).build()
    nc = _CACHE[key]
    x = np.asarray(inputs["x"], np.float32)
    consts, oh = host_consts()
    vecs = host_vecs(inputs)
    shared = {
        "vecs": vecs, "consts": consts, "onehot": oh,
        "rel_bias": np.ascontiguousarray(np.asarray(inputs["rel_bias"], np.float32)),
        "ab_w_in": np.ascontiguousarray(np.asarray(inputs["ab_w_in"], np.float32)[0]),
        "pool_w": np.ascontiguousarray(np.asarray(inputs["pool_w"], np.float32)[0]),
        "ab_w_out": np.ascontiguousarray(np.asarray(inputs["ab_w_out"], np.float32)[0]),
        "c_w_in": np.ascontiguousarray(np.asarray(inputs["c_w_in"], np.float32)[0]),
        "c_w_out": np.ascontiguousarray(np.asarray(inputs["c_w_out"], np.float32)[0]),
    }
    for nm in ("ffn1", "ffn2"):
        for w in ("w_gate", "w_up", "w_down"):
            shared[f"{nm}_{w}"] = np.ascontiguousarray(np.asarray(inputs[f"{nm}_{w}"], np.float32))
    in_maps = []
    for c in range(n_cores):
        m = dict(shared)
        m["x"] = np.ascontiguousarray(x[c * n_seq:(c + 1) * n_seq].reshape(n_seq * S, D))
        in_maps.append(m)
    res = run_bass_kernel_spmd(nc, in_maps, core_ids=list(range(n_cores)), trace=trace)
    out = np.stack([r["out"].reshape(n_seq, S, D) for r in res.results], 0)
    return out.reshape(n_cores * n_seq, S, D), res


def kernel(**inputs):
    out, _ = run(inputs, n_seq=4, stages=6, do_final=True, n_cores=8)
    return out.astype(np.float32)
```

```python
import math, os
from contextlib import ExitStack
import numpy as np
import concourse.bass as bass
import concourse.mybir as mybir
from concourse.bass_utils import run_bass_kernel_spmd

F32 = mybir.dt.float32
BF16 = mybir.dt.bfloat16
ALU = mybir.AluOpType
AF = mybir.ActivationFunctionType
AX = mybir.AxisListType

D = 1024
S = 2048
DFF = 2816
NF = 22
CH = 1024
NTT = CH // 512
EPS = 1e-6
NIT = 12
FVL = 1152
NEG = -1.0e30

C_ID, C_J, C_CM, C_ONE, C_P2, C_IC = 0, 128, 256, 384, 512, 528
NCONST = 544
V_G = 0
V_PS = 56
V_CW = 60
V_CB = 184
V_LG = 188
V_LB = 192
V_B31 = 196
NVEC = 212


class Node:
    __slots__ = ("eng", "fn", "deps", "sig", "val", "dma", "semi", "waits")


class Sched:
    COMPUTE = ("pe", "act", "dve", "pool")
    KD = 8

    def __init__(self):
        self.nodes = []
        self.recs = {}

    def add(self, eng, fn, reads=(), writes=(), dma=False):
        nid = len(self.nodes)
        n = Node()
        n.eng, n.fn, n.dma, n.sig, n.val, n.semi = eng, fn, dma, False, 0, -1
        deps = {}
        recs = self.recs
        for (b, lo, hi) in reads:
            for r in recs.get(b, ()):
                if r[3] and r[0] < hi and lo < r[1]:
                    deps[r[2]] = True
        for (b, lo, hi) in writes:
            for r in recs.get(b, ()):
                if r[0] < hi and lo < r[1] and r[2] not in deps:
                    deps[r[2]] = False
        keep = []
        nodes = self.nodes
        for d, raw in deps.items():
            dn = nodes[d]
            if dn.eng == eng and not dn.dma and not dma:
                if eng == "pe":
                    continue
            keep.append(d)
        n.deps = keep
        for (b, lo, hi) in writes:
            L = recs.setdefault(b, [])
            L[:] = [r for r in L if not (lo <= r[0] and r[1] <= hi)]
            L.append((lo, hi, nid, True))
        for (b, lo, hi) in reads:
            L = recs.setdefault(b, [])
            if not dma:
                L[:] = [r for r in L if not ((not r[3]) and lo <= r[0] and r[1] <= hi
                                             and nodes[r[2]].eng == eng and not nodes[r[2]].dma)]
            L.append((lo, hi, nid, False))
        nodes.append(n)
        return nid

    def finalize(self):
        nodes = self.nodes
        for n in nodes:
            for d in n.deps:
                nodes[d].sig = True
        cnt = {e: 0 for e in self.COMPUTE}
        dcnt = {}
        for n in nodes:
            if n.dma:
                j = dcnt.get(n.eng, 0)
                dcnt[n.eng] = j + 1
                n.semi = j % self.KD
                n.val = 16 * (j // self.KD + 1)
            elif n.sig:
                cnt[n.eng] += 1
                n.val = cnt[n.eng]
        waited = {}
        for n in nodes:
            w = {}
            for d in n.deps:
                dn = nodes[d]
                key = ("d", dn.eng, dn.semi) if dn.dma else ("c", dn.eng)
                if dn.val > w.get(key, 0):
                    w[key] = dn.val
            if n.dma and n.val > 16:
                key = ("d", n.eng, n.semi)
                w[key] = max(w.get(key, 0), n.val - 16)
            out = []
            for key, v in w.items():
                wk = (n.eng, key)
                if waited.get(wk, 0) >= v:
                    continue
                waited[wk] = v
                out.append((key, v))
            n.waits = out

    def emit(self, nc, block, sems):
        engmap = {"pe": block.tensor, "act": block.scalar, "dve": block.vector,
                  "pool": block.gpsimd, "sp": block.sync}
        nodes = self.nodes
        for ename, deco in engmap.items():
            mine = [n for n in nodes if n.eng == ename]
            if not mine:
                continue

            def body(e, mine=mine, ename=ename):
                for n in mine:
                    for key, v in n.waits:
                        e.wait_ge(sems[key], v)
                    ins = n.fn(e)
                    if n.dma:
                        ins.then_inc(sems[("d", ename, n.semi)], 16)
                    elif n.sig:
                        ins.then_inc(sems[("c", ename)], 1)
                last = {}
                for n in mine:
                    if n.dma:
                        last[n.semi] = n.val
                for semi, v in last.items():
                    e.wait_ge(sems[("d", ename, semi)], v)
            deco(body)


class V:
    __slots__ = ("ap", "reg")

    def __init__(self, ap, reg):
        self.ap, self.reg = ap, reg


class Buf:
    def __init__(self, space, base_ap, off_words, shape, dtype):
        self.space = space
        self.esz = 4 if dtype == F32 else 2
        self.shape = tuple(shape)
        nel = int(np.prod(shape))
        self.words = (nel * self.esz + 3) // 4
        self.off = off_words
        v = base_ap[:, off_words:off_words + self.words]
        if dtype != F32:
            v = v.bitcast(dtype)
        if len(shape) == 2:
            v = v.rearrange("p (a b) -> p a b", a=shape[0])
        elif len(shape) == 3:
            v = v.rearrange("p (a b c) -> p a b c", a=shape[0], b=shape[1])
        self.ap = v
        st = []
        acc = 1
        for s_ in reversed(shape):
            st.append(acc)
            acc *= s_
        self.strides = tuple(reversed(st))

    def v(self, *idx, p=None):
        lo = 0
        hi = 0
        key = []
        for i, s_ in enumerate(self.shape):
            it = idx[i] if i < len(idx) else None
            if it is None:
                a, b = 0, s_
                key.append(slice(None))
            elif isinstance(it, tuple):
                a, b = it
                key.append(slice(a, b))
            else:
                a, b = it, it + 1
                key.append(it)
            lo += a * self.strides[i]
            hi += (b - 1) * self.strides[i]
        hi += 1
        ps = slice(None) if p is None else slice(p[0], p[1])
        ap = self.ap[(ps,) + tuple(key)]
        reg = (self.space, self.off + (lo * self.esz) // 4, self.off + (hi * self.esz + 3) // 4)
        return V(ap, reg)


class Prog:
    def __init__(self, n_seq, stages, do_final):
        self.n_seq = n_seq
        self.stages = stages
        self.do_final = do_final
        self.S = Sched()
        self.nc = bass.Bass("TRN2", target_bir_lowering=False)
        self.psi = 0
        self.units = []
        self.rr = 0

    def mm(self, out, lhsT, rhs, start, stop):
        self.S.add("pe", lambda e, o=out.ap, l=lhsT.ap, r=rhs.ap, a=start, b=stop:
                   e.matmul(o, l, r, start=a, stop=b),
                   reads=[lhsT.reg, rhs.reg], writes=[out.reg])

    def tr(self, out, in_, ident):
        self.S.add("pe", lambda e, o=out.ap, i=in_.ap, d=ident.ap: e.transpose(o, i, d),
                   reads=[in_.reg, ident.reg], writes=[out.reg])

    def act(self, out, in_, func, bias=0.0, scale=1.0, accum=None, eng="act"):
        reads = [in_.reg]
        b = bias
        s = scale
        if isinstance(bias, V):
            reads.append(bias.reg)
            b = bias.ap
        if isinstance(scale, V):
            reads.append(scale.reg)
            s = scale.ap
        writes = [out.reg]
        kw = {}
        if accum is not None:
            writes.append(accum.reg)
            kw["accum_out"] = accum.ap
        self.S.add("act", lambda e, o=out.ap, i=in_.ap, f=func, b=b, s=s, kw=kw:
                   e.activation(o, i, f, bias=b, scale=s, **kw), reads=reads, writes=writes)

    def tt(self, eng, out, in0, in1, op):
        self.S.add(eng, lambda e, o=out.ap, a=in0.ap, b=in1.ap, op=op: e.tensor_tensor(o, a, b, op),
                   reads=[in0.reg, in1.reg], writes=[out.reg])

    def ts(self, eng, out, in0, s1, s2, op0, op1=None, accum=None):
        reads = [in0.reg]
        a1, a2 = s1, s2
        if isinstance(s1, V):
            reads.append(s1.reg)
            a1 = s1.ap
        if isinstance(s2, V):
            reads.append(s2.reg)
            a2 = s2.ap
        writes = [out.reg]
        kw = {}
        if op1 is not None:
            kw["op1"] = op1
        if accum is not None:
            writes.append(accum.reg)
            kw["accum_out"] = accum.ap
        self.S.add(eng, lambda e, o=out.ap, i=in0.ap, a1=a1, a2=a2, op0=op0, kw=kw:
                   e.tensor_scalar(o, i, a1, a2, op0, **kw), reads=reads, writes=writes)

    def stt(self, out, in0, sc, in1, op0, op1):
        reads = [in0.reg, in1.reg]
        a = sc
        if isinstance(sc, V):
            reads.append(sc.reg)
            a = sc.ap
        self.S.add("dve", lambda e, o=out.ap, i0=in0.ap, a=a, i1=in1.ap, op0=op0, op1=op1:
                   e.scalar_tensor_tensor(o, i0, a, i1, op0, op1), reads=reads, writes=[out.reg])

    def copy(self, eng, out, in_):
        if eng == "act":
            self.S.add("act", lambda e, o=out.ap, i=in_.ap: e.copy(o, i), reads=[in_.reg], writes=[out.reg])
        else:
            self.S.add(eng, lambda e, o=out.ap, i=in_.ap: e.tensor_copy(o, i), reads=[in_.reg], writes=[out.reg])

    def recip(self, out, in_):
        self.S.add("dve", lambda e, o=out.ap, i=in_.ap: e.reciprocal(o, i), reads=[in_.reg], writes=[out.reg])

    def memset(self, eng, out, val):
        self.S.add(eng, lambda e, o=out.ap, v=val: e.memset(o, v), reads=[], writes=[out.reg])

    def dma(self, eng, out_ap, in_ap, reads, writes, slow=False):
        kw = {"allow_slow_non_contiguous": True} if slow else {}
        self.S.add(eng, lambda e, o=out_ap, i=in_ap, kw=kw: e.dma_start(out=o, in_=i, **kw),
                   reads=reads, writes=writes, dma=True)

    def evac(self, out, in_):
        self.rr ^= 1
        self.copy("act" if self.rr else "dve", out, in_)

    def psn(self, excl=()):
        i = self.psi
        while i in excl:
            i = (i + 1) % 8
        self.psi = (i + 1) % 8
        return i

    def pb(self, i, n=512, p=None):
        return self.PS[i].v((0, n), p=p)

    def build(self):
        nc = self.nc
        n_seq = self.n_seq
        ntok = n_seq * S
        dt = lambda name, shape, kind="ExternalInput", d=F32: nc.dram_tensor(name, shape, d, kind=kind).ap()
        self.x_d = dt("x", [ntok, D])
        self.out_d = dt("out", [ntok, D], kind="ExternalOutput")
        self.wd = {}
        for nm in ("ffn1", "ffn2"):
            self.wd[nm + "_g"] = dt(nm + "_w_gate", [2, D, DFF])
            self.wd[nm + "_u"] = dt(nm + "_w_up", [2, D, DFF])
            self.wd[nm + "_d"] = dt(nm + "_w_down", [2, DFF, D])
        self.wd["ab_in"] = dt("ab_w_in", [D, 1536])
        self.wd["pool_w"] = dt("pool_w", [4, 128, 128])
        self.wd["ab_out"] = dt("ab_w_out", [D, D])
        self.wd["c_in"] = dt("c_w_in", [D, 3656])
        self.wd["c_out"] = dt("c_w_out", [D, D])
        self.relb_d = dt("rel_bias", [32, 16])
        self.vecs_d = dt("vecs", [128, NVEC])
        self.consts_d = dt("consts", [128, NCONST])
        self.oh_d = dt("onehot", [32, FVL])
        self.fvh_d = dt("fvscrh", [16, FVL], kind="Internal", d=BF16)
        self.fvl_d = dt("fvscrl", [16, FVL], kind="Internal", d=BF16)
        self.bw_d = dt("bwscr", [16, 128, 1024], kind="Internal")
        self.ks_d = [dt(f"kscr{c}", [128, S], kind="Internal", d=BF16) for c in range(8)]
        self.vs_d = [dt(f"vscr{c}", [S, 128], kind="Internal", d=BF16) for c in range(8)]

        with ExitStack() as st:
            TOTW = 212000 // 4 // 8 * 8
            arena = st.enter_context(nc.sbuf_tensor("arena", [128, TOTW], F32))
            self.PS = []
            for i in range(8):
                p = st.enter_context(nc.psum_tensor(f"ps{i}", [128, 512], F32))
                self.PS.append(Buf(f"ps{i}", p, 0, (512,), F32))
                self.PS[-1].raw = p
            sems = {}
            for e in Sched.COMPUTE:
                sems[("c", e)] = st.enter_context(nc.semaphore(f"c_{e}"))
            for e in ("sp", "pool"):
                for k in range(Sched.KD):
                    sems[("d", e, k)] = st.enter_context(nc.semaphore(f"d_{e}{k}"))

            off = [0]

            def alloc(shape, dtype):
                b = Buf("sb", arena, off[0], shape, dtype)
                off[0] += (b.words + 7) // 8 * 8
                return b
            self.arena = arena
            self.consts = alloc((NCONST,), F32)
            self.vecs = alloc((NVEC,), F32)
            self.cbf = alloc((384,), BF16)
            self.relb = alloc((16,), F32)
            self.xT = alloc((8, CH), F32)
            self.hT = alloc((8, CH), BF16)
            self.ring = [alloc((4096,), BF16) for _ in range(4)]
            self.ki2 = alloc((S,), BF16)
            self.small = alloc((128,), F32)
            self.halo_u = alloc((4, 16), F32)
            self.halo_z = alloc((4, 32), F32)
            self.A0 = off[0]
            self.AW = TOTW - self.A0
            assert self.AW >= 26500, self.AW

            self.make_units()
            self.prologue()
            self.convert_all()
            nchunks = n_seq * (S // CH)
            self.wpos = 0
            self.wuse = 0
            self.load_x(0)
            for ci in range(nchunks):
                self.chunk(ci, nchunks)
            self.S.finalize()
            with nc.Block() as block:
                self.S.emit(nc, block, sems)
        return nc

    def ab(self, off_words, shape, dtype):
        return Buf("sb", self.arena, self.A0 + off_words, shape, dtype)

    def prologue(self):
        self.dma("sp", self.consts.ap, self.consts_d, [], [self.consts.v().reg])
        self.dma("sp", self.vecs.ap, self.vecs_d, [], [self.vecs.v().reg])
        self.dma("sp", self.relb.v(p=(0, 32)).ap, self.relb_d, [], [self.relb.v().reg])
        self.copy("dve", self.cbf.v((0, 128)), self.consts.v((C_ID, C_ID + 128)))
        self.copy("dve", self.cbf.v((128, 256)), self.consts.v((C_ONE, C_ONE + 128)))
        self.copy("dve", self.cbf.v((256, 384)), self.consts.v((C_J, C_J + 128)))
        self.Jb = self.cbf.v((256, 384))
        self.ident = self.consts.v((C_ID, C_ID + 128))
        self.identb = self.cbf.v((0, 128))
        self.onesb = self.cbf.v((128, 256))
        self.onesf = self.consts.v((C_ONE, C_ONE + 128))
        if self.stages >= 2:
            um = {u["name"]: u for u in self.units}
            for j in range(4):
                dg = self.ab((j % 2) * 2048, (31, 128), BF16)
                for tap in range(31):
                    self.ts("dve", dg.v(tap), self.identb,
                            self.vecs.v((V_CW + j * 31 + tap, V_CW + j * 31 + tap + 1)), None, ALU.mult)
                self.dma("sp", um[f"cdiag{j}"]["dram"], dg.ap.rearrange("p a b -> p (a b)"), [dg.v().reg],
                         [(f"w_cdiag{j}", 0, 1)])
        if self.stages >= 5:
            oh = self.ab(0, (FVL,), F32)
            fvs = self.ab(1160, (FVL,), F32)
            ohb = self.ab(2320, (FVL,), BF16)
            fvh = self.ab(2900, (FVL,), BF16)
            fvf = self.ab(3480, (FVL,), F32)
            fvl = self.ab(4640, (FVL,), BF16)
            rbh = self.ab(5220, (16,), BF16)
            rbf = self.ab(5230, (16,), F32)
            rbl = self.ab(5250, (16,), BF16)
            P32 = (0, 32)
            P16 = (0, 16)
            self.dma("sp", oh.v(p=P32).ap, self.oh_d, [], [oh.v().reg])
            self.copy("dve", ohb.v(p=P32), oh.v(p=P32))
            self.copy("dve", rbh.v(p=P32), self.relb.v(p=P32))
            self.copy("dve", rbf.v(p=P32), rbh.v(p=P32))
            self.tt("dve", rbl.v(p=P32), self.relb.v(p=P32), rbf.v(p=P32), ALU.subtract)
            for b0 in range(0, FVL, 512):
                w = min(512, FVL - b0)
                pi = self.psn()
                self.mm(self.pb(pi, w, p=P16), rbh.v(p=P32), ohb.v((b0, b0 + w), p=P32), True, False)
                self.mm(self.pb(pi, w, p=P16), rbl.v(p=P32), ohb.v((b0, b0 + w), p=P32), False, True)
                self.copy("dve", fvs.v((b0, b0 + w), p=P16), self.pb(pi, w, p=P16))
            self.copy("dve", fvh.v(p=P16), fvs.v(p=P16))
            self.copy("dve", fvf.v(p=P16), fvh.v(p=P16))
            self.tt("dve", fvl.v(p=P16), fvs.v(p=P16), fvf.v(p=P16), ALU.subtract)
            self.dma("sp", self.fvh_d, fvh.v(p=P16).ap, [fvh.v().reg], [("fvh", 0, 1)])
            self.dma("sp", self.fvl_d, fvl.v(p=P16).ap, [fvl.v().reg], [("fvl", 0, 1)])
            for head in range(16):
                Wh = self.ab(6000 + (head % 2) * 1024, (1024,), BF16)
                Wl = self.ab(6000 + (head % 2) * 1024 + 512, (1024,), BF16)
                bwt = self.ab(8048 + (head % 2) * 1024, (1024,), F32)
                for (Wx, fd, nm) in ((Wh, self.fvh_d, "fvh"), (Wl, self.fvl_d, "fvl")):
                    src = bass.AP(tensor=fd.tensor, offset=head * FVL, ap=[[1, 128], [1, 1024]])
                    self.dma("sp", Wx.ap, src, [(nm, 0, 1)], [Wx.v().reg])
                for hf in range(2):
                    pi = self.psn()
                    self.mm(self.pb(pi), self.Jb, Wh.v((hf * 512, hf * 512 + 512)), True, False)
                    self.mm(self.pb(pi), self.Jb, Wl.v((hf * 512, hf * 512 + 512)), False, True)
                    self.evac(bwt.v((hf * 512, hf * 512 + 512)), self.pb(pi))
                self.dma("sp", self.bw_d[head], bwt.ap, [bwt.v().reg], [("bw", head, head + 1)])

    def make_units(self):
        U = []
        wd = self.wd

        def colunit(name, mat, cols):
            tot = sum(n for _, n in cols)
            parts = []
            o = 0
            src = mat.rearrange("(k p) f -> p k f", p=128)
            for lo, n in cols:
                parts.append((o, n, src[:, :, lo:lo + n]))
                o += n
            U.append(dict(name=name, size=8 * tot, kind="col", tot=tot, parts=parts))

        def ffn(l, nm):
            for u in range(11):
                parts = []
                for gi, key in enumerate(("_g", "_u")):
                    src = wd[nm + key][l].rearrange("(k p) f -> p k f", p=128)[:, :, 256 * u:256 * u + 256]
                    parts.append((gi * 2048, 256, src))
                U.append(dict(name=f"{nm}{l}gu{u}", size=4096, kind="gu", parts=parts))
            for c in range(8):
                src = wd[nm + "_d"][l].rearrange("(f p) d -> p f d", p=128)[:, :, 128 * c:128 * c + 128]
                U.append(dict(name=f"{nm}{l}d{c}", size=NF * 128, kind="dn", parts=[(0, 128, src)]))
        st = self.stages
        if st >= 1:
            ffn(0, "ffn1")
        if st >= 2:
            colunit("abin0", wd["ab_in"], [(0, 512)])
            colunit("abin1", wd["ab_in"], [(512, 256), (1024, 256)])
            colunit("abin2", wd["ab_in"], [(768, 256), (1280, 256)])
            U.append(dict(name="poolw", size=512, kind="pw",
                          parts=[(0, 128, wd["pool_w"].rearrange("g c d -> c g d"))]))
            for j in range(4):
                U.append(dict(name=f"cdiag{j}", size=3968, kind="pre", parts=[]))
            colunit("about0", wd["ab_out"], [(0, 512)])
            colunit("about1", wd["ab_out"], [(512, 512)])
        if st >= 3:
            ffn(0, "ffn2")
        if st >= 4:
            ffn(1, "ffn1")
        if st >= 5:
            colunit("cqi", wd["c_in"], [(3072, 512)])
            colunit("ckw", wd["c_in"], [(3584, 64), (3584, 64), (3648, 8)])
            for c in range(8):
                colunit(f"cqkv{c}", wd["c_in"], [(128 * c, 128), (1024 + 128 * c, 128), (2048 + 128 * c, 128)])
            colunit("cout0", wd["c_out"], [(0, 512)])
            colunit("cout1", wd["c_out"], [(512, 512)])
        if st >= 6:
            ffn(1, "ffn2")
        self.units = U
        for i, u in enumerate(U):
            u["dram"] = self.nc.dram_tensor("w_" + u["name"], [128, u["size"]], BF16, kind="Internal").ap()
            u["idx"] = i

    def convert_all(self):
        for u in self.units:
            d = u["dram"]
            reg = ("w_" + u["name"], 0, 1)
            if u["kind"] == "col":
                dv = d.rearrange("p (k f) -> p k f", k=8)
                for (o, n, src) in u["parts"]:
                    self.dma("pool", dv[:, :, o:o + n], src, [], [reg])
            elif u["kind"] == "gu":
                for (o, n, src) in u["parts"]:
                    self.dma("pool", d[:, o:o + 2048].rearrange("p (k f) -> p k f", k=8), src, [], [reg])
            elif u["kind"] == "pre":
                pass
            elif u["kind"] == "dn":
                self.dma("pool", d.rearrange("p (f d) -> p f d", f=NF), u["parts"][0][2], [], [reg])
            else:
                self.dma("pool", d.rearrange("p (g d) -> p g d", g=4), u["parts"][0][2], [], [reg])

    def wload_upto(self, gidx):
        nu = len(self.units)
        total = nu * self.n_seq * (S // CH)
        while self.wpos <= gidx and self.wpos < total:
            u = self.units[self.wpos % nu]
            slot = self.ring[self.wpos % 4]
            self.dma("sp", slot.v((0, u["size"])).ap, u["dram"], [("w_" + u["name"], 0, 1)],
                     [slot.v((0, u["size"])).reg])
            self.wpos += 1

    def wget(self, name):
        nu = len(self.units)
        g = self.wuse
        u = self.units[g % nu]
        while u["name"] != name and "DBG_CUT" in os.environ:
            self.wload_upto(g + 3)
            self.wuse += 1
            g = self.wuse
            u = self.units[g % nu]
        assert u["name"] == name, (u["name"], name)
        self.wload_upto(g + 3)
        self.wuse += 1
        return self.ring[g % 4]

    def stage_buf(self):
        return self.ab(self.AW - 8192 - 8, (8, D), F32)

    def load_x(self, ci):
        stg = self.stage_buf()
        r0 = ci * CH
        self.dma("sp", stg.ap, self.x_d[r0:r0 + CH, :].rearrange("(t p) d -> p t d", p=128),
                 [], [stg.v().reg])

    def transpose_in(self):
        stg = self.stage_buf()
        for k in range(8):
            for tb in range(2):
                pi = self.psn()
                for j in range(4):
                    self.tr(self.PS[pi].v((j * 128, j * 128 + 128)),
                            stg.v(tb * 4 + j, (k * 128, k * 128 + 128)), self.ident)
                self.evac(self.xT.v(k, (tb * 512, tb * 512 + 512)), self.pb(pi))

    def gcol(self, vi, k):
        return self.vecs.v((V_G + vi * 8 + k, V_G + vi * 8 + k + 1))

    def norm_stats(self, n, sqoff):
        sq = self.ab(sqoff, (8, 512), BF16)
        sd = self.ab(sqoff + 2048, (512,), F32)
        rstd = self.ab(sqoff + 2048 + 512, (512,), F32)
        self.act(sq.v(), self.xT.v(None, (n * 512, n * 512 + 512)), AF.Square)
        pi = self.psn()
        for k in range(8):
            self.mm(self.pb(pi), self.onesb, sq.v(k), k == 0, k == 7)
        self.act(sd.v(), self.pb(pi), AF.Sqrt, bias=self.epsb, scale=1.0 / D)
        self.recip(rstd.v(), sd.v())
        return rstd

    def rmsnorm(self, vi, sqoff):
        for n in range(NTT):
            rstd = self.norm_stats(n, sqoff)
            for k in range(8):
                self.stt(self.hT.v(k, (n * 512, n * 512 + 512)), self.xT.v(k, (n * 512, n * 512 + 512)),
                         self.gcol(vi, k), rstd.v(), ALU.mult, ALU.mult)

    def final_out(self, ci):
        ost = self.ab(11264, (8, D), F32)
        yb = [self.ab(i * 512, (512,), F32) for i in range(2)]
        sqoff = 1024
        for n in range(NTT):
            if self.do_final:
                rstd = self.norm_stats(n, sqoff)
            for k in range(8):
                y = yb[k % 2]
                xs = self.xT.v(k, (n * 512, n * 512 + 512))
                if self.do_final:
                    self.stt(y.v(), xs, self.gcol(6, k), rstd.v(), ALU.mult, ALU.mult)
                else:
                    self.copy("dve", y.v(), xs)
                pi = self.psn()
                for j in range(4):
                    self.tr(self.PS[pi].v((j * 128, j * 128 + 128)), y.v((j * 128, j * 128 + 128)), self.ident)
                pv = V(self.PS[pi].ap.rearrange("p (a b) -> p a b", a=4), self.PS[pi].v().reg)
                self.evac(ost.v((n * 4, n * 4 + 4), (k * 128, k * 128 + 128)), pv)
        r0 = ci * CH
        self.dma("sp", self.out_d[r0:r0 + CH, :].rearrange("(t p) d -> p t d", p=128), ost.ap,
                 [ost.v().reg], [("out", r0, r0 + CH)])

    def ffn(self, l, nm, vi):
        aT = self.ab(0, (NF, CH), BF16)
        sg = [self.ab(11264 + i * 256, (512,), BF16) for i in range(2)]
        self.rmsnorm(vi, 11264 + 512)
        for u in range(11):
            wt = self.wget(f"{nm}{l}gu{u}")
            for f2 in range(2):
                f = 2 * u + f2
                for n in range(NTT):
                    pg = self.psn()
                    pu = self.psn()
                    hs = lambda k: self.hT.v(k, (n * 512, n * 512 + 512))
                    for k in range(8):
                        o = k * 256 + f2 * 128
                        self.mm(self.pb(pg), wt.v((o, o + 128)), hs(k), k == 0, k == 7)
                    for k in range(8):
                        o = 2048 + k * 256 + f2 * 128
                        self.mm(self.pb(pu), wt.v((o, o + 128)), hs(k), k == 0, k == 7)
                    s = sg[(f * NTT + n) % 2]
                    self.act(s.v(), self.pb(pg), AF.Silu)
                    self.tt("dve", aT.v(f, (n * 512, n * 512 + 512)), s.v(), self.pb(pu), ALU.mult)
        for c in range(8):
            wt = self.wget(f"{nm}{l}d{c}")
            for n in range(NTT):
                po = self.psn()
                for f in range(NF):
                    self.mm(self.pb(po), wt.v((f * 128, f * 128 + 128)), aT.v(f, (n * 512, n * 512 + 512)),
                            f == 0, f == NF - 1)
                xs = self.xT.v(c, (n * 512, n * 512 + 512))
                self.stt(xs, self.pb(po), 0.5, xs, ALU.mult, ALU.add)

    def chunk(self, ci, nchunks):
        h = ci % (S // CH)
        self.transpose_in()
        st = self.stages
        last = None
        phases = []
        if st >= 1:
            phases.append(lambda: self.ffn(0, "ffn1", 0))
        if st >= 2:
            phases.append(lambda: self.mixer0(h))
        if st >= 3:
            phases.append(lambda: self.ffn(0, "ffn2", 2))
        if st >= 4:
            phases.append(lambda: self.ffn(1, "ffn1", 3))
        if st >= 5:
            phases.append(lambda: self.attn(h))
        if st >= 6:
            phases.append(lambda: self.ffn(1, "ffn2", 5))
        pref = st in (1, 3, 4, 6)
        for i, ph in enumerate(phases):
            if pref and i == len(phases) - 1 and ci + 1 < nchunks:
                self.load_x(ci + 1)
            ph()
        self.final_out(ci)
        if not pref and ci + 1 < nchunks:
            self.load_x(ci + 1)

    def mixer0(self, h):
        up = self.ab(0, (4, 1040), F32)
        tA = self.ab(4160, (1040,), F32)
        tB = self.ab(5200, (1040,), F32)
        zc = self.ab(0, (4, 1024), F32)
        z = self.ab(6240, (4, 1056), BF16)
        dif = self.ab(10464, (1024,), BF16)
        sgm = [self.ab(10976 + i * 512, (512,), F32) for i in range(2)]
        sqoff = 12000
        sqc = self.ab(12000, (4, 512), F32)
        sdl = self.ab(12000 + 2048, (512,), F32)
        rsl = self.ab(12000 + 2560, (512,), F32)
        yab = self.hT
        T = lambda n: (n * 512, n * 512 + 512)
        self.rmsnorm(1, sqoff)
        if h == 0:
            self.memset("dve", up.v(None, (0, 16)), 0.0)
            self.memset("dve", z.v(None, (0, 32)), 0.0)
        else:
            self.copy("dve", up.v(None, (0, 16)), self.halo_u.v())
            self.copy("dve", z.v(None, (0, 32)), self.halo_z.v())
        wt = self.wget("abin0")
        for g in range(4):
            for n in range(NTT):
                pi = self.psn()
                for k in range(8):
                    o = k * 512 + g * 128
                    self.mm(self.pb(pi), wt.v((o, o + 128)), self.hT.v(k, T(n)), k == 0, k == 7)
                self.evac(up.v(g, (16 + n * 512, 16 + n * 512 + 512)), self.pb(pi))
        for ui in range(2):
            wt = self.wget(f"abin{ui + 1}")
            for jj in range(2):
                j = 2 * ui + jj
                for n in range(NTT):
                    pv = self.psn()
                    pg = self.psn()
                    for k in range(8):
                        o = k * 512 + jj * 128
                        self.mm(self.pb(pv), wt.v((o, o + 128)), self.hT.v(k, T(n)), k == 0, k == 7)
                    for k in range(8):
                        o = k * 512 + 256 + jj * 128
                        self.mm(self.pb(pg), wt.v((o, o + 128)), self.hT.v(k, T(n)), k == 0, k == 7)
                    sg = sgm[(j * NTT + n) % 2]
                    self.act(sg.v(), self.pb(pg), AF.Sigmoid)
                    self.tt("dve", z.v(j, (32 + n * 512, 32 + n * 512 + 512)), sg.v(), self.pb(pv), ALU.mult)
        CUT = int(os.environ.get("DBG_CUT", "99"))
        if CUT <= 3:
            return
        if h == 0:
            self.copy("dve", self.halo_u.v(), up.v(None, (1024, 1040)))
            self.copy("dve", self.halo_z.v(), z.v(None, (1024, 1056)))
        pw = self.wget("poolw")
        for g in range(4):
            w = 2 ** (g + 1)
            src = None
            bufs = [tA, tB]
            for lv in range(1, g + 2):
                sh = 2 ** (lv - 1)
                lo = 2 ** lv
                dst = bufs[lv % 2]
                if lv == 1:
                    self.tt("dve", dst.v((lo, 1040)), up.v(g, (lo, 1040)), up.v(g, (lo - sh, 1040 - sh)), ALU.add)
                else:
                    self.tt("dve", dst.v((lo, 1040)), src.v((lo, 1040)), src.v((lo - sh, 1040 - sh)), ALU.add)
                src = dst
            self.stt(dif.v(), src.v((16, 1040)), 1.0 / w, up.v(g, (16, 1040)), ALU.mult, ALU.subtract)
            if h == 0:
                oth = bufs[(g + 2) % 2]
                self.tt("dve", oth.v((0, w - 1)), src.v((16, 16 + w - 1)),
                        self.consts.v((C_IC, C_IC + w - 1)), ALU.mult)
                self.tt("dve", dif.v((0, w - 1)), oth.v((0, w - 1)), up.v(g, (16, 16 + w - 1)), ALU.subtract)
            for n in range(NTT):
                pi = self.psn()
                self.mm(self.pb(pi), pw.v((g * 128, g * 128 + 128)), dif.v(T(n)), True, True)
                self.ts("dve", yab.v(g, T(n)), self.pb(pi), self.vecs.v((V_PS + g, V_PS + g + 1)), None, ALU.mult)
        if CUT <= 4:
            return
        for j in range(4):
            wt = self.wget(f"cdiag{j}")
            cb = self.vecs.v((V_CB + j, V_CB + j + 1))
            for n in range(NTT):
                pi = self.psn()
                for tap in range(31):
                    o = 2 + tap + n * 512
                    self.mm(self.pb(pi), wt.v((tap * 128, tap * 128 + 128)), z.v(j, (o, o + 512)), tap == 0, tap == 30)
                self.act(zc.v(j, T(n)), self.pb(pi), AF.Identity, bias=cb)
        if CUT <= 5:
            return
        sqb = self.ab(12000, (4, 512), BF16)
        zcb = self.ab(12000 + 1024, (4, 512), BF16)
        for n in range(NTT):
            pm = self.psn()
            self.copy("act", zcb.v(), zc.v(None, T(n)))
            for j in range(4):
                self.mm(self.pb(pm), self.onesb, zcb.v(j), j == 0, j == 3)
            for j in range(4):
                self.stt(zc.v(j, T(n)), self.pb(pm), -1.0 / 512, zc.v(j, T(n)), ALU.mult, ALU.add)
            self.act(sqb.v(), zc.v(None, T(n)), AF.Square)
            pv = self.psn()
            for j in range(4):
                self.mm(self.pb(pv), self.onesb, sqb.v(j), j == 0, j == 3)
            self.act(sdl.v(), self.pb(pv), AF.Sqrt, bias=self.epsb, scale=1.0 / 512)
            self.recip(rsl.v(), sdl.v())
            for j in range(4):
                self.tt("dve", zc.v(j, T(n)), zc.v(j, T(n)), rsl.v(), ALU.mult)
                self.act(yab.v(4 + j, T(n)), zc.v(j, T(n)), AF.Silu,
                         bias=self.vecs.v((V_LB + j, V_LB + j + 1)), scale=self.vecs.v((V_LG + j, V_LG + j + 1)))
        if CUT <= 6:
            return
        for ui in range(2):
            wt = self.wget(f"about{ui}")
            for cc in range(4):
                c = ui * 4 + cc
                for n in range(NTT):
                    pi = self.psn()
                    for k in range(8):
                        o = k * 512 + cc * 128
                        self.mm(self.pb(pi), wt.v((o, o + 128)), yab.v(k, T(n)), k == 0, k == 7)
                    self.tt("dve", self.xT.v(c, T(n)), self.xT.v(c, T(n)), self.pb(pi), ALU.add)

    def attn(self, h):
        T0 = CH * h
        ab = self.ab
        qT = ab(0, (8, CH), BF16)
        qiT = ab(4096, (4, CH), BF16)
        maskT = ab(6144, (16, 512), BF16)
        sc = ab(10240, (S,), F32)
        mrow = ab(12288, (S,), BF16)
        kbuf = [ab(13312 + i * 1024, (S,), BF16) for i in range(2)]
        vbuf = [ab(15360 + i * 1024, (16, 128), BF16) for i in range(2)]
        kst = [ab(17408 + i * 512, (CH,), BF16) for i in range(2)]
        vst = [ab(18432 + i * 512, (8, 128), BF16) for i in range(2)]
        Whs = [ab(19456, (1024,), BF16), ab(25216, (1024,), BF16)]
        Wls = [ab(19456 + 512, (1024,), BF16), ab(25216 + 512, (1024,), BF16)]
        biasWs = [ab(20480, (1024,), F32), ab(26240, (1024,), F32)]
        pT = [ab(21504 + i * 256, (512,), BF16) for i in range(4)]
        tmpf = [ab(22528 + i * 512, (512,), F32) for i in range(2)]
        rbuf = [ab(23552 + i * 512, (512,), F32) for i in range(2)]
        rdb = ab(24576, (512,), F32)
        wts = ab(25088, (64,), F32)
        sm = ab(25152, (64,), F32)
        sqoff = 6144
        attnT = self.hT
        A = sm.v((0, 1))
        thr = [sm.v((1, 2)), sm.v((2, 3))]
        cnt = sm.v((3, 4))
        gg = sm.v((4, 5))
        thrneg = sm.v((5, 6))
        stp = Buf("sb", self.arena, self.A0 + 25152 + 8, (NIT,), F32)
        st2 = Buf("sb", self.arena, self.A0 + 25152 + 8 + NIT, (NIT,), F32)
        T = lambda n: (n * 512, n * 512 + 512)
        self.rmsnorm(4, sqoff)
        self.memset("dve", thrneg, -1.0e29)
        wt = self.wget("cqi")
        for c4 in range(4):
            for n in range(NTT):
                pi = self.psn()
                for k in range(8):
                    o = k * 512 + c4 * 128
                    self.mm(self.pb(pi), wt.v((o, o + 128)), self.hT.v(k, T(n)), k == 0, k == 7)
                self.evac(qiT.v(c4, T(n)), self.pb(pi))
        wt = self.wget("ckw")
        for n in range(NTT):
            pi = self.psn()
            for k in range(8):
                self.mm(self.pb(pi), wt.v((k * 136, k * 136 + 128)), self.hT.v(k, T(n)), k == 0, k == 7)
            self.evac(self.ki2.v((T0 + n * 512, T0 + n * 512 + 512)), self.pb(pi))
        pi = self.psn()
        for tt in range(8):
            for k in range(8):
                self.mm(self.PS[pi].v((tt * 8, tt * 8 + 8)), self.hT.v(k, (tt * 128, tt * 128 + 128)),
                        wt.v((k * 136 + 128, k * 136 + 136)), k == 0, k == 7)
        self.copy("dve", wts.v(), self.PS[pi].v((0, 64)))
        wabs = ab(27264, (64,), F32)
        wsg = ab(27264 + 64, (64,), F32)
        dgs = ab(27264 + 128, (8, 128), BF16)
        rb = [ab(23552 + i * 256, (512,), BF16) for i in range(4)]
        dgs2 = ab(27264 + 128 + 512, (8, 128), BF16)
        smB = ab(27264 + 128 + 1024, (64,), F32)
        sc2 = ab(13312, (S,), F32)
        qz = [[ab(28480 + (a * 2 + b) * 256, (512,), BF16) for b in range(2)] for a in range(2)]
        for a in range(2):
            self.memset("pool", qz[a][0].v(p=(64, 128)), 0.0)
            self.memset("pool", qz[a][1].v(p=(0, 64)), 0.0)
        mrow2 = ab(15360, (S,), BF16)
        self.act(wsg.v(), wts.v(), AF.Sign)
        self.tt("dve", wabs.v(), wts.v(), wsg.v(), ALU.mult)
        for c in range(8):
            wt = self.wget(f"cqkv{c}")
            for n in range(NTT):
                pi = self.psn()
                for k in range(8):
                    self.mm(self.pb(pi), wt.v((k * 384, k * 384 + 128)), self.hT.v(k, T(n)), k == 0, k == 7)
                self.evac(qT.v(c, T(n)), self.pb(pi))
            ks = kst[c % 2]
            for n in range(NTT):
                pi = self.psn()
                for k in range(8):
                    self.mm(self.pb(pi), wt.v((k * 384 + 128, k * 384 + 256)), self.hT.v(k, T(n)), k == 0, k == 7)
                self.evac(ks.v(T(n)), self.pb(pi))
            self.dma("sp", self.ks_d[c][:, T0:T0 + CH], ks.ap, [ks.v().reg], [(f"kscr{c}", T0, T0 + CH)])
            vs = vst[c % 2]
            for hf in range(2):
                pi = self.psn()
                for j4 in range(4):
                    tt = hf * 4 + j4
                    for k in range(8):
                        self.mm(self.PS[pi].v((j4 * 128, j4 * 128 + 128)), self.hT.v(k, (tt * 128, tt * 128 + 128)),
                                wt.v((k * 384 + 256, k * 384 + 384)), k == 0, k == 7)
                pv = V(self.PS[pi].ap.rearrange("p (a b) -> p a b", a=4), self.PS[pi].v().reg)
                self.evac(vs.v((hf * 4, hf * 4 + 4)), pv)
            self.dma("sp", self.vs_d[c][T0:T0 + CH, :].rearrange("(j p) d -> p j d", p=128), vs.ap,
                     [vs.v().reg], [(f"vscr{c}", T0, T0 + CH)])
        for tb in range(2):
            B = 2 * h + tb
            for t4p in (0, 2):
                tiles = []
                for ti in range(2):
                    t4 = t4p + ti
                    tt = tb * 4 + t4
                    I = 8 * h + tt
                    L = 128 * (I + 1)
                    scb = sc if ti == 0 else sc2
                    mr = mrow if ti == 0 else mrow2
                    smx = sm if ti == 0 else smB
                    A_ = smx.v((0, 1))
                    thr_ = [smx.v((1, 2)), smx.v((2, 3))]
                    cnt_ = smx.v((3, 4))
                    gg_ = smx.v((4, 5))
                    stp_ = Buf("sb", self.arena, smx.off + 8, (NIT,), F32)
                    st2_ = Buf("sb", self.arena, smx.off + 8 + NIT, (NIT,), F32)
                    tiles.append((t4, tt, I, L, scb, mr, A_, thr_, cnt_, gg_, stp_, st2_))
                    nblk = (L + 511) // 512
                    dg = dgs if ti == 0 else dgs2
                    for hh in range(8):
                        self.ts("dve", dg.v(hh), self.identb, wsg.v((tt * 8 + hh, tt * 8 + hh + 1)), None, ALU.mult)
                    for sb in range(nblk):
                        wd_ = min(512, L - 512 * sb)
                        scs = scb.v((sb * 512, sb * 512 + wd_))
                        pacc = 4 + (sb % 2)

                        def zq(hh):
                            pr = (64 * (hh % 2), 64 * (hh % 2) + 64)
                            self.mm(self.pb(hh % 4, wd_), qiT.v(hh // 2, (tt * 128, tt * 128 + 128), p=pr),
                                    self.ki2.v((sb * 512, sb * 512 + wd_), p=pr), True, True)
                        zq(0)
                        zq(1)
                        for hh in range(8):
                            r = rb[hh % 4]
                            self.act(r.v((0, wd_)), self.pb(hh % 4, wd_), AF.Relu,
                                     scale=wabs.v((tt * 8 + hh, tt * 8 + hh + 1)))
                            if hh + 2 < 8:
                                zq(hh + 2)
                            self.mm(self.pb(pacc, wd_), dg.v(hh), r.v((0, wd_)), hh == 0, hh == 7)
                        self.copy("dve", scs, self.pb(pacc, wd_))
                    if I >= 2:
                        self.S.add("dve", lambda e, o=A_.ap, i=scb.v((0, L)).ap:
                                   e.tensor_reduce(o, i, AX.X, ALU.max, apply_absolute_value=True),
                                   reads=[scb.v((0, L)).reg], writes=[A_.reg])
                    self.tt("dve", scb.v((L - 128, L)), scb.v((L - 128, L)), self.consts.v((C_CM, C_CM + 128)), ALU.add)
                    if I >= 2:
                        self.ts("dve", stp_.v(), self.consts.v((C_P2, C_P2 + NIT)), A_, None, ALU.mult)
                        self.ts("dve", st2_.v(), stp_.v(), 2.0, None, ALU.mult)
                        self.memset("dve", thr_[0], 0.0)
                for k in range(NIT):
                    for (t4, tt, I, L, scb, mr, A_, thr_, cnt_, gg_, stp_, st2_) in tiles:
                        if I < 2:
                            continue
                        cur, nxt = thr_[k % 2], thr_[(k + 1) % 2]
                        self.ts("dve", mr.v((0, L)), scb.v((0, L)), cur, None, ALU.is_ge, ALU.add, accum=cnt_)
                        self.ts("dve", gg_, cnt_, 255.5, st2_.v((k, k + 1)), ALU.is_ge, ALU.mult)
                        self.stt(nxt, gg_, stp_.v((k, k + 1)), cur, ALU.subtract, ALU.add)
                for (t4, tt, I, L, scb, mr, A_, thr_, cnt_, gg_, stp_, st2_) in tiles:
                    thf = thr_[NIT % 2] if I >= 2 else thrneg
                    self.ts("dve", mr.v((0, L)), scb.v((0, L)), thf, None, ALU.is_ge)
                    for j0 in range(0, I + 1, 4):
                        nj = min(4, I + 1 - j0)
                        pi = 6 + ((j0 // 4) % 2)
                        psb = self.PS[pi].ap.bitcast(BF16)
                        preg = self.PS[pi].v().reg
                        for jj in range(nj):
                            self.tr(V(psb[:, jj * 128:jj * 128 + 128], preg),
                                    mr.v(((j0 + jj) * 128, (j0 + jj) * 128 + 128)), self.identb)
                        src = V(psb[:, 0:nj * 128].rearrange("p (a b) -> p a b", a=nj), preg)
                        self.evac(maskT.v((j0, j0 + nj), (t4 * 128, t4 * 128 + 128)), src)
                    if I + 1 < 4 * B + 4:
                        self.memset("pool", maskT.v((I + 1, 4 * B + 4), (t4 * 128, t4 * 128 + 128)), 0.0)
            nj_all = 4 * B + 4
            Lb = 128 * nj_all
            def bias_build(head):
                bW = biasWs[head % 2]
                self.dma("sp", bW.ap, self.bw_d[head], [("bw", head, head + 1)], [bW.v().reg])

            def kv_load(c):
                kb = kbuf[c % 2]
                vb = vbuf[c % 2]
                self.dma("sp", kb.v((0, Lb)).ap, self.ks_d[c][:, 0:Lb], [(f"kscr{c}", 0, Lb)], [kb.v((0, Lb)).reg])
                self.dma("sp", vb.v((0, nj_all)).ap, self.vs_d[c][0:Lb, :].rearrange("(j p) d -> p j d", p=128),
                         [(f"vscr{c}", 0, Lb)], [vb.v((0, nj_all)).reg])

            LA = 3
            items = [(c, hd, j) for c in range(8) for hd in range(2) for j in range(nj_all)]
            PR = lambda hd: (64 * hd, 64 * hd + 64)

            def qz_build(c):
                for hd in range(2):
                    self.copy("pool", qz[c % 2][hd].v(p=PR(hd)), qT.v(c, T(tb), p=PR(hd)))

            def qk(idx):
                c, hd, j = items[idx]
                self.mm(self.pb(4 + idx % 4), kbuf[c % 2].v((j * 128, j * 128 + 128)),
                        qz[c % 2][hd].v(), True, True)
            kv_load(0)
            qz_build(0)
            bias_build(0)
            for idx in range(min(LA, len(items))):
                qk(idx)
            for idx, (c, hd, j) in enumerate(items):
                head = 2 * c + hd
                pr = PR(hd)
                vb = vbuf[c % 2]
                biasW = biasWs[head % 2]
                po, pd = (0, 1) if head % 2 == 0 else (2, 3)
                if j == 0:
                    if hd == 0 and c + 1 < 8:
                        kv_load(c + 1)
                        qz_build(c + 1)
                    if head + 1 < 16:
                        bias_build(head + 1)
                pS = 4 + idx % 4
                r = j - 4 * B
                pt = pT[idx % 4]
                if r >= -1:
                    tf = tmpf[idx % 2]
                    o = 384 - 128 * r
                    self.stt(tf.v(), self.pb(pS), 0.125, biasW.v((o, o + 512)), ALU.mult, ALU.add)
                    self.act(pt.v(), tf.v(), AF.Exp)
                else:
                    self.act(pt.v(), self.pb(pS), AF.Exp,
                             bias=self.vecs.v((V_B31 + head, V_B31 + head + 1)), scale=0.125)
                self.tt("pool" if idx % 2 else "dve", pt.v(), pt.v(), maskT.v(j), ALU.mult)
                if idx + LA < len(items):
                    qk(idx + LA)
                self.mm(self.pb(po), vb.v(j), pt.v(), j == 0, j == nj_all - 1)
                self.mm(self.pb(pd), self.onesb, pt.v(), j == 0, j == nj_all - 1)
                if j == nj_all - 1:
                    self.recip(rdb.v(p=pr), self.pb(pd, p=pr))
                    self.tt("dve", attnT.v(c, T(tb), p=pr), self.pb(po, p=pr), rdb.v(p=pr), ALU.mult)
        for ui in range(2):
            wt = self.wget(f"cout{ui}")
            for cc in range(4):
                c = ui * 4 + cc
                for n in range(NTT):
                    pi = self.psn()
                    for k in range(8):
                        o = k * 512 + cc * 128
                        self.mm(self.pb(pi), wt.v((o, o + 128)), attnT.v(k, T(n)), k == 0, k == 7)
                    self.tt("dve", self.xT.v(c, T(n)), self.xT.v(c, T(n)), self.pb(pi), ALU.add)


def _eps_setup(P):
    P.epsb = P.small.v((0, 1))
    P.memset("dve", P.epsb, EPS)


_orig_prologue = Prog.prologue


def _prologue(self):
    _eps_setup(self)
    _orig_prologue(self)


Prog.prologue = _prologue


def t5_bucket_np(dist):
    max_exact = 16
    d = np.maximum(dist, max_exact).astype(np.float32)
    large = max_exact + (np.log(d / max_exact) / math.log(128 / max_exact) * 16).astype(np.int32)
    large = np.minimum(large, 31)
    return np.where(dist < max_exact, dist, large)


def host_consts():
    c = np.zeros((128, NCONST), np.float32)
    c[:, C_ID:C_ID + 128] = np.eye(128, dtype=np.float32)
    c[:, C_J:C_J + 128] = np.eye(128, dtype=np.float32)[::-1]
    t = np.arange(128)[:, None]
    s = np.arange(128)[None, :]
    c[:, C_CM:C_CM + 128] = np.where(s <= t, 0.0, NEG)
    c[:, C_ONE:C_ONE + 128] = 1.0
    c[:, C_P2:C_P2 + NIT] = (0.5 ** np.arange(1, NIT + 1))[None, :]
    c[:, C_IC:C_IC + 16] = (1.0 / np.arange(1, 17))[None, :]
    oh = np.zeros((32, FVL), np.float32)
    dd = np.arange(FVL) - 511
    ok = dd >= 0
    b = t5_bucket_np(np.maximum(dd, 0))
    oh[b[ok], np.arange(FVL)[ok]] = 1.0
    return c, oh


def host_vecs(inp):
    v = np.zeros((128, NVEC), np.float32)
    gl = [inp["ffn1_norm"][0], inp["mix_norm"][0], inp["ffn2_norm"][0],
          inp["ffn1_norm"][1], inp["mix_norm"][1], inp["ffn2_norm"][1], inp["final_norm"]]
    for i, g in enumerate(gl):
        v[:, V_G + i * 8:V_G + i * 8 + 8] = np.asarray(g, np.float32).reshape(8, 128).T
    v[:, V_PS:V_PS + 4] = np.asarray(inp["pool_scale"][0], np.float32).reshape(4, 128).T
    cw = np.asarray(inp["conv_w"][0], np.float32)
    v[:, V_CW:V_CW + 124] = cw.reshape(31, 4, 128).transpose(2, 1, 0).reshape(128, 124)
    v[:, V_CB:V_CB + 4] = np.asarray(inp["conv_b"][0], np.float32).reshape(4, 128).T
    v[:, V_LG:V_LG + 4] = np.asarray(inp["conv_ln_g"][0], np.float32).reshape(4, 128).T
    v[:, V_LB:V_LB + 4] = np.asarray(inp["conv_ln_b"][0], np.float32).reshape(4, 128).T
    v[:, V_B31:V_B31 + 16] = np.asarray(inp["rel_bias"], np.float32)[31][None, :]
    return v


_CACHE = {}


def run(inputs, n_seq=4, stages=6, do_final=True, n_cores=8, trace=False):
    key = (n_seq, stages, do_final)
    if key not in _CACHE:
        _CACHE[key] = Prog(n_seq, stages, do_final).build()
    nc = _CACHE[key]
    x = np.asarray(inputs["x"], np.float32)
    consts, oh = host_consts()
    vecs = host_vecs(inputs)
    shared = {
        "vecs": vecs, "consts": consts, "onehot": oh,
        "rel_bias": np.ascontiguousarray(np.asarray(inputs["rel_bias"], np.float32)),
        "ab_w_in": np.ascontiguousarray(np.asarray(inputs["ab_w_in"], np.float32)[0]),
        "pool_w": np.ascontiguousarray(np.asarray(inputs["pool_w"], np.float32)[0]),
        "ab_w_out": np.ascontiguousarray(np.asarray(inputs["ab_w_out"], np.float32)[0]),
        "c_w_in": np.ascontiguousarray(np.asarray(inputs["c_w_in"], np.float32)[0]),
        "c_w_out": np.ascontiguousarray(np.asarray(inputs["c_w_out"], np.float32)[0]),
    }
    for nm in ("ffn1", "ffn2"):
        for w in ("w_gate", "w_up", "w_down"):
            shared[f"{nm}_{w}"] = np.ascontiguousarray(np.asarray(inputs[f"{nm}_{w}"], np.float32))
    in_maps = []
    for c in range(n_cores):
        m = dict(shared)
        m["x"] = np.ascontiguousarray(x[c * n_seq:(c + 1) * n_seq].reshape(n_seq * S, D))
        in_maps.append(m)
    res = run_bass_kernel_spmd(nc, in_maps, core_ids=list(range(n_cores)), trace=trace)
    out = np.stack([r["out"].reshape(n_seq, S, D) for r in res.results], 0)
    return out.reshape(n_cores * n_seq, S, D), res


def kernel(**inputs):
    out, _ = run(inputs, n_seq=4, stages=6, do_final=True, n_cores=8)
    return out.astype(np.float32)
```
